# Optimizing a Trainium2 kernel written in Bass

```python
import jax, jax.numpy as jnp
from jax import lax
import numpy as np

D_MODEL = 2048
BATCH = 4
SEQ = 2048
DEPTH = 2
DEC_BATCH = 8
DEC_SEQ = 4
PAST_LEN = 16384
PAGE_SIZE = 128

N_EVEN = (DEPTH + 1) // 2
N_ODD = DEPTH // 2
A_HEADS = 4
A_DH = D_MODEL // 8
A_WIDTH = A_HEADS * A_DH
A_CHUNK = 128
B_HEADS = 8
B_DH = D_MODEL // 16
B_WIDTH = B_HEADS * B_DH
Q_BLOCK = 128
C_WIDTH = D_MODEL
C_GROUPS = 8
C_GDIM = C_WIDTH // C_GROUPS
C_CHUNK = 128
EVEN_MIX = A_WIDTH + B_WIDTH
EVEN_IN = 5 * A_WIDTH + 2 * A_HEADS + 4 * B_WIDTH
ODD_IN = 3 * C_WIDTH
RMS_EPS = 1e-6

kernel_name = "hybrid_mlstm_stickbreak_chunkgmlp_step"


def rmsnorm(x, g):
    xf = x.astype(jnp.float32)
    y = xf * lax.rsqrt(jnp.mean(xf * xf, axis=-1, keepdims=True) + RMS_EPS) * g.astype(jnp.float32)
    return y.astype(x.dtype)


def _split_points(sizes):
    pts, acc = [], 0
    for s in sizes[:-1]:
        acc += s
        pts.append(acc)
    return pts


def mlstm_chunk(state, chunk):
    c0, n0, m0 = state
    q, k, v, ig, lf = chunk
    L = q.shape[2]
    b = jnp.cumsum(lf, axis=-1)
    causal = jnp.tril(jnp.ones((L, L), dtype=bool))
    d = jnp.where(causal, b[..., :, None] - b[..., None, :] + ig[..., None, :], -jnp.inf)
    m_carry = b + m0[..., None]
    m = jnp.maximum(m_carry, jnp.max(d, axis=-1))
    w_intra = jnp.exp(d - m[..., None])
    w_carry = jnp.exp(m_carry - m)
    s = jnp.einsum('bhtd,bhsd->bhts', q, k) * w_intra
    num = jnp.einsum('bhts,bhsd->bhtd', s, v) + w_carry[..., None] * jnp.einsum('bhvk,bhtk->bhtv', c0, q)
    den = jnp.sum(s, axis=-1) + w_carry * jnp.einsum('bhk,bhtk->bht', n0, q)
    h = num / jnp.maximum(jnp.abs(den), jnp.exp(-m))[..., None]
    m_new = m[..., -1]
    w_last = jnp.exp(d[..., -1, :] - m_new[..., None])
    wc_last = w_carry[..., -1]
    c_new = wc_last[..., None, None] * c0 + jnp.einsum('bhs,bhsv,bhsk->bhvk', w_last, v, k)
    n_new = wc_last[..., None] * n0 + jnp.einsum('bhs,bhsk->bhk', w_last, k)
    return (c_new, n_new, m_new), h


def mlstm_seq(q, k, v, ig, lf, state):
    t_len = q.shape[2]
    L = min(A_CHUNK, t_len)
    if t_len % L:
        L = t_len
    nc = t_len // L

    def to_chunks(t):
        return jnp.moveaxis(t.reshape(t.shape[:2] + (nc, L) + t.shape[3:]), 2, 0)

    chunks = (to_chunks(q), to_chunks(k), to_chunks(v), to_chunks(ig), to_chunks(lf))
    state, h = lax.scan(mlstm_chunk, state, chunks)
    h = jnp.moveaxis(h, 0, 2).reshape(q.shape)
    return h, state


def stick_breaking(q, k, v, q_pos, k_pos, b_sb):
    z = (jnp.einsum('bqhd,bkhd->bhqk', q, k).astype(jnp.float32) * (B_DH ** -0.5)
         + b_sb.astype(jnp.float32)[None, :, None, None])
    mask = k_pos[None, :] < q_pos[:, None]
    log_1m = jnp.where(mask, jax.nn.log_sigmoid(-z), 0.0)
    rest = lax.cumsum(log_1m, axis=3, reverse=True) - log_1m
    a = jnp.where(mask, jnp.exp(jax.nn.log_sigmoid(z) + rest), 0.0)
    return jnp.einsum('bhqk,bkhd->bqhd', a.astype(v.dtype), v)


def stick_breaking_blocks(q, k, v, q_start, k_pos, b_sb):
    bsz, tq, nh, dh = q.shape
    blk = min(Q_BLOCK, tq)
    nb = -(-tq // blk)
    pad = nb * blk - tq
    qp = jnp.pad(q, ((0, 0), (0, pad), (0, 0), (0, 0)))
    qb = qp.reshape(bsz, nb, blk, nh, dh).transpose(1, 0, 2, 3, 4)
    pos = (q_start + jnp.arange(nb * blk)).reshape(nb, blk)
    out = lax.map(lambda a: stick_breaking(a[0], k, v, a[1], k_pos, b_sb), (qb, pos))
    return out.transpose(1, 0, 2, 3, 4).reshape(bsz, nb * blk, nh, dh)[:, :tq]


def even_mixer(x, g_norm, w_in, b_i, b_f, b_sb, w_out, a_state, past_kv, q_start):
    bsz, t_len, _ = x.shape
    h = rmsnorm(x, g_norm)
    sizes = [A_WIDTH] * 5 + [A_HEADS] * 2 + [B_WIDTH] * 4
    qa, ka, va, oa, ga, ia, fa, qb, kb, vb, gb = jnp.split(h @ w_in, _split_points(sizes), axis=-1)

    def a_heads(t):
        return t.reshape(bsz, t_len, A_HEADS, A_DH).transpose(0, 2, 1, 3).astype(jnp.float32)

    ig = (ia + b_i).astype(jnp.float32).transpose(0, 2, 1)
    lf = jax.nn.log_sigmoid((fa + b_f).astype(jnp.float32)).transpose(0, 2, 1)
    if a_state is None:
        a_state = (jnp.zeros((bsz, A_HEADS, A_DH, A_DH), jnp.float32),
                   jnp.zeros((bsz, A_HEADS, A_DH), jnp.float32),
                   jnp.zeros((bsz, A_HEADS), jnp.float32))
    else:
        a_state = (a_state[0].astype(jnp.float32), a_state[1].astype(jnp.float32),
                   a_state[2].astype(jnp.float32))
    h_a, new_state = mlstm_seq(a_heads(qa), a_heads(ka) * (A_DH ** -0.5), a_heads(va), ig, lf, a_state)
    h_a = h_a.transpose(0, 2, 1, 3).reshape(bsz, t_len, A_WIDTH).astype(x.dtype) * jax.nn.sigmoid(oa)

    qb4 = qb.reshape(bsz, t_len, B_HEADS, B_DH)
    kb4 = kb.reshape(bsz, t_len, B_HEADS, B_DH)
    vb4 = vb.reshape(bsz, t_len, B_HEADS, B_DH)
    if past_kv is None:
        k_all, v_all = kb4, vb4
    else:
        k_all = jnp.concatenate([past_kv[0].astype(kb4.dtype), kb4], axis=1)
        v_all = jnp.concatenate([past_kv[1].astype(vb4.dtype), vb4], axis=1)
    k_pos = jnp.arange(k_all.shape[1])
    h_b = stick_breaking_blocks(qb4, k_all, v_all, q_start, k_pos, b_sb).reshape(bsz, t_len, B_WIDTH)

    y = jnp.concatenate([h_a * jax.nn.silu(ga), h_b * jax.nn.silu(gb)], axis=-1) @ w_out
    return x + y, new_state, kb4, vb4


def chunk_spatial_gate(u, v, w_s, b_s):
    bsz, t_len, _ = v.shape
    nc = -(-t_len // C_CHUNK)
    pad = nc * C_CHUNK - t_len
    vp = jnp.pad(v, ((0, 0), (0, pad), (0, 0))).reshape(bsz, nc, C_CHUNK, C_GROUPS, C_GDIM)
    causal = jnp.tril(jnp.ones((C_CHUNK, C_CHUNK), dtype=bool))
    wm = jnp.where(causal, w_s, 0.0).astype(v.dtype)
    sv = jnp.einsum('gts,bnsgc->bntgc', wm, vp) + b_s.T.astype(v.dtype)[None, None, :, :, None]
    sv = sv.reshape(bsz, nc * C_CHUNK, C_WIDTH)[:, :t_len]
    return u * sv


def odd_mixer(x, g_norm, w_in, v_gain, w_s, b_s, w_out):
    h = rmsnorm(x, g_norm)
    u, v, g = jnp.split(h @ w_in, 3, axis=-1)
    u = jax.nn.gelu(u)
    v = rmsnorm(jax.nn.gelu(v), v_gain)
    y = chunk_spatial_gate(u, v, w_s, b_s) * jax.nn.silu(g)
    return x + y @ w_out, v


def setup_inputs(seed: int = 0) -> dict:
    key = jax.random.key(seed)
    ks = jax.random.split(key, 24)
    n_pages = PAST_LEN // PAGE_SIZE
    n_used = DEC_BATCH * n_pages
    n_pool = (5 * n_used + 3) // 4
    f32 = jnp.float32
    x_prompt = jax.random.normal(ks[0], (BATCH, SEQ, D_MODEL), f32)
    x_sample = jax.random.normal(ks[1], (DEC_BATCH, DEC_SEQ, D_MODEL), f32)
    state_a_C = 0.05 * jax.random.normal(ks[2], (N_EVEN, DEC_BATCH, A_HEADS, A_DH, A_DH), f32)
    state_a_n = 0.1 * jax.random.normal(ks[3], (N_EVEN, DEC_BATCH, A_HEADS, A_DH), f32)
    state_a_m = jax.random.normal(ks[4], (N_EVEN, DEC_BATCH, A_HEADS), f32)
    cache_b_k = jax.random.normal(ks[5], (N_EVEN, n_pool, PAGE_SIZE, B_HEADS, B_DH), f32)
    cache_b_v = jax.random.normal(ks[6], (N_EVEN, n_pool, PAGE_SIZE, B_HEADS, B_DH), f32)
    page_table = jax.random.permutation(ks[7], n_pool)[:n_used].reshape(DEC_BATCH, n_pages).astype(jnp.int32)
    even_norm = 1.0 + 0.01 * jax.random.normal(ks[8], (N_EVEN, D_MODEL), f32)
    even_w_in = jax.random.normal(ks[9], (N_EVEN, D_MODEL, EVEN_IN), f32) * (D_MODEL ** -0.5)
    even_b_i = 0.1 * jax.random.normal(ks[10], (N_EVEN, A_HEADS), f32)
    even_b_f = jnp.linspace(3.0, 6.0, A_HEADS, dtype=f32)[None, :] + 0.1 * jax.random.normal(ks[11], (N_EVEN, A_HEADS), f32)
    even_b_sb = jnp.linspace(-4.0, -10.0, B_HEADS, dtype=f32)[None, :] + 0.1 * jax.random.normal(ks[20], (N_EVEN, B_HEADS), f32)
    even_w_out = jax.random.normal(ks[12], (N_EVEN, EVEN_MIX, D_MODEL), f32) * (EVEN_MIX ** -0.5)
    odd_norm = 1.0 + 0.01 * jax.random.normal(ks[13], (N_ODD, D_MODEL), f32)
    odd_w_in = jax.random.normal(ks[14], (N_ODD, D_MODEL, ODD_IN), f32) * (D_MODEL ** -0.5)
    odd_v_gain = 1.0 + 0.01 * jax.random.normal(ks[15], (N_ODD, C_WIDTH), f32)
    odd_w_s = jax.random.normal(ks[16], (N_ODD, C_GROUPS, C_CHUNK, C_CHUNK), f32) * (C_CHUNK ** -0.5)
    odd_b_s = 1.0 + 0.1 * jax.random.normal(ks[17], (N_ODD, C_GROUPS, C_CHUNK), f32)
    odd_w_out = jax.random.normal(ks[18], (N_ODD, C_WIDTH, D_MODEL), f32) * (C_WIDTH ** -0.5)
    final_norm = 1.0 + 0.01 * jax.random.normal(ks[19], (D_MODEL,), f32)
    return {"x_prompt": x_prompt, "x_sample": x_sample,
            "state_a_C": state_a_C, "state_a_n": state_a_n, "state_a_m": state_a_m,
            "cache_b_k": cache_b_k, "cache_b_v": cache_b_v, "page_table": page_table,
            "even_norm": even_norm, "even_w_in": even_w_in, "even_b_i": even_b_i,
            "even_b_f": even_b_f, "even_b_sb": even_b_sb, "even_w_out": even_w_out,
            "odd_norm": odd_norm, "odd_w_in": odd_w_in, "odd_v_gain": odd_v_gain,
            "odd_w_s": odd_w_s, "odd_b_s": odd_b_s, "odd_w_out": odd_w_out,
            "final_norm": final_norm}


def reference(x_prompt, x_sample, state_a_C, state_a_n, state_a_m, cache_b_k, cache_b_v, page_table,
              even_norm, even_w_in, even_b_i, even_b_f, even_b_sb, even_w_out,
              odd_norm, odd_w_in, odd_v_gain, odd_w_s, odd_b_s, odd_w_out, final_norm):
    n_seq, n_pages = page_table.shape
    past_len = n_pages * PAGE_SIZE
    xp, xs = x_prompt, x_sample
    aCp, anp, amp, aCs, ans, ams = [], [], [], [], [], []
    bkp, bvp, bks, bvs, cvs = [], [], [], [], []
    for layer in range(DEPTH):
        j = layer // 2
        if layer % 2 == 0:
            w = (even_norm[j], even_w_in[j], even_b_i[j], even_b_f[j], even_b_sb[j], even_w_out[j])
            xp, st_p, k_p, v_p = even_mixer(xp, *w, None, None, 0)
            past_k = jnp.take(cache_b_k[j], page_table, axis=0).reshape(n_seq, past_len, B_HEADS, B_DH)
            past_v = jnp.take(cache_b_v[j], page_table, axis=0).reshape(n_seq, past_len, B_HEADS, B_DH)
            st_in = (state_a_C[j], state_a_n[j], state_a_m[j])
            xs, st_s, k_s, v_s = even_mixer(xs, *w, st_in, (past_k, past_v), past_len)
            aCp.append(st_p[0]); anp.append(st_p[1]); amp.append(st_p[2])
            aCs.append(st_s[0]); ans.append(st_s[1]); ams.append(st_s[2])
            bkp.append(k_p); bvp.append(v_p); bks.append(k_s); bvs.append(v_s)
        else:
            w = (odd_norm[j], odd_w_in[j], odd_v_gain[j], odd_w_s[j], odd_b_s[j], odd_w_out[j])
            xp, _ = odd_mixer(xp, *w)
            xs, v_rows = odd_mixer(xs, *w)
            cvs.append(v_rows)
    y_prompt = rmsnorm(xp, final_norm)
    y_sample = rmsnorm(xs, final_norm)
    return (y_prompt, y_sample,
            jnp.stack(aCp), jnp.stack(anp), jnp.stack(amp),
            jnp.stack(aCs), jnp.stack(ans), jnp.stack(ams),
            jnp.stack(bkp), jnp.stack(bvp), jnp.stack(bks), jnp.stack(bvs),
            jnp.stack(cvs))
```

```python
import os
import numpy as np
import concourse.bass as bass
import concourse.mybir as mybir
from concourse.bass_utils import run_bass_kernel_spmd
from contextlib import ExitStack

F32 = mybir.dt.float32
BF16 = mybir.dt.bfloat16
I32 = mybir.dt.int32
AF = mybir.ActivationFunctionType
ALU = mybir.AluOpType
AX = mybir.AxisListType.X

NEG = -30000.0
EPS = 1e-6
C_ID, C_TLE, C_ONE, C_CM, C_NTG, C_NON, C_NM, C_DM = 0, 128, 256, 384, 512, 640, 768, 800
NCST = 800 + 2048
P_G0, P_G1, P_VG, P_BI, P_BF, P_BSB, P_BSBS, P_FLAG, P_CTX, P_AM, P_PIDX = 0, 16, 32, 48, 52, 56, 64, 96, 97, 98, 102
NPAR = 104

CENG = ('pe', 'act', 'dve', 'pool')


class Sched:
    def __init__(self, nc, stack):
        self.nc = nc
        self.eng = {'pe': nc.tensor, 'act': nc.scalar, 'dve': nc.vector, 'pool': nc.gpsimd, 'sp': nc.sync}
        self.sem = {e: stack.enter_context(nc.semaphore("sem_" + e)) for e in CENG}
        self.cnt = {e: 0 for e in CENG}
        self.known = {e: {} for e in self.eng}
        self.lastw = {}
        self.readers = {}
        self.dsem = {}
        self.dcnt = {}
        self.stack = stack
        self.final = []
        self.n_ops = 0

    def _wait(self, eng, tok):
        kind, f, val = tok
        if kind == 'E' and f == eng and eng == 'pe':
            return
        kk = (kind, f)
        if self.known[eng].get(kk, 0) >= val:
            return
        self.known[eng][kk] = val
        s = self.sem[f] if kind == 'E' else self.dsem[f]
        self.eng[eng].wait_ge(s, val)

    @staticmethod
    def _norm(k):
        if isinstance(k, str):
            if k.startswith('pm_'):
                return 'pm'
            if k in ('ptb0',):
                return 'ptb'
            if k in ('pz', 'pz2', 'pz3'):
                return 'pO'
            return k
        if k[0] in ('ptr', 'ptr2'):
            return (k[0], k[1])
        if k[0] == 'ptb1':
            return 'ptb'
        if k[0] == 'pos':
            return ('pP', k[1])
        return k

    @staticmethod
    def _is_psum(k):
        if isinstance(k, str):
            return k in ('pm', 'ptb', 'pO', 'pin', 'pit')
        return k[0] in ('ptr', 'ptr2', 'pP', 'pO', 'pzb', 'posb', 'pu', 'pj', 'pkt', 'pj4', 'pa', 'pb', 'pc')

    def op(self, eng, fn, reads=(), writes=(), dma=None, final=False):
        reads = [self._norm(k) for k in reads]
        writes = [self._norm(k) for k in writes]
        pr = [k for k in reads if self._is_psum(k)]
        if pr:
            writes = list(writes) + [k for k in pr if k not in writes]
        deps = []
        for k in reads:
            t = self.lastw.get(k)
            if t is not None:
                deps.append(t)
        for k in writes:
            t = self.lastw.get(k)
            if t is not None:
                deps.append(t)
            deps.extend(self.readers.get(k, {}).values())
        for t in deps:
            self._wait(eng, t)
        ins = fn(self.eng[eng])
        self.n_ops += 1
        if dma is not None:
            if dma not in self.dsem:
                self.dsem[dma] = self.stack.enter_context(self.nc.semaphore("d_" + str(len(self.dsem))))
                self.dcnt[dma] = 0
            self.dcnt[dma] += 16
            ins.then_inc(self.dsem[dma], 16)
            tok = ('D', dma, self.dcnt[dma])
        else:
            self.cnt[eng] += 1
            ins.then_inc(self.sem[eng], 1)
            tok = ('E', eng, self.cnt[eng])
        for k in writes:
            self.lastw[k] = tok
            self.readers[k] = {}
        for k in reads:
            self.readers.setdefault(k, {})[(tok[0], tok[1])] = tok
        if final:
            self.final.append(tok)
        return tok

    def barrier(self):
        toks = [('E', e, self.cnt[e]) for e in CENG if self.cnt[e] > 0]
        toks += [('D', k, v) for k, v in self.dcnt.items()]
        for e in self.eng:
            for t in toks:
                self._wait(e, t)

    def finish(self):
        for t in self.final:
            self._wait('sp', t)


def build(npool, stage=99):
    nc = bass.Bass("TRN2", target_bir_lowering=False)

    def din(name, shape, dt=F32):
        return nc.dram_tensor(name, shape, dt, kind="ExternalInput").ap()

    def dout(name, shape):
        return nc.dram_tensor(name, shape, F32, kind="ExternalOutput").ap()

    xw = din("xw", [2048, 2048]); xs = din("xs", [4, 2048])
    MINI = os.environ.get("K_MINI", "0") == "1"
    w_in0 = din("w_in0", [2048, 16 if MINI else 9224]); w_out0 = din("w_out0", [2048, 16 if MINI else 2048])
    w_in1 = din("w_in1", [2048, 16 if MINI else 6144]); w_out1 = din("w_out1", [2048, 16 if MINI else 2048])
    cst_d = din("cst", [128, NCST]); par_d = din("par", [128, NPAR])
    bsbc_d = din("bsbc", [128, 1024]); vgbc_d = din("vgbc", [128, 2048]); gfbc_d = din("gfbc", [128, 2048])
    wsT_d = din("wsT", [128, 8, 128]); aCT_d = din("aCT", [4, 256, 257])
    ck_d = din("ck", [npool * 128, 1024]); cv_d = din("cv", [npool * 128, 1024])
    ptb_d = din("ptb", [128, 128], I32)

    y_p = dout("y_p", [1024, 2048]); y_s = dout("y_s", [4, 2048])
    CTo_p = dout("CTo_p", [4, 256, 257]); mo_p = dout("mo_p", [1, 4])
    CTo_s = dout("CTo_s", [4, 256, 257]); mo_s = dout("mo_s", [1, 4])
    bk_p = dout("bk_p", [1024, 1024]); bv_p = dout("bv_p", [1024, 1024])
    bk_s = dout("bk_s", [4, 1024]); bv_s = dout("bv_s", [4, 1024]); cv_s = dout("cv_s", [4, 2048])

    top = ExitStack()
    S = Sched(nc, top)

    def sbt(stack, name, shape, dt=F32):
        return stack.enter_context(nc.sbuf_tensor("s_" + name, shape, dt))

    def pst(stack, name, shape, dt=F32):
        return stack.enter_context(nc.psum_tensor("p_" + name, shape, dt))

    def mm(out, lhsT, rhs, start, stop, r, w):
        S.op('pe', lambda e: e.matmul(out, lhsT, rhs, start=start, stop=stop), r, w)

    def tr(out, in_, ident, r, w):
        S.op('pe', lambda e: e.transpose(out, in_, ident), r, w)

    def act(out, in_, func, r, w, bias=None, scale=None, accum=None):
        kw = {}
        if bias is not None: kw['bias'] = bias
        if scale is not None: kw['scale'] = scale
        if accum is not None: kw['accum_out'] = accum
        S.op('act', lambda e: e.activation(out=out, in_=in_, func=func, **kw), r, w)

    def tt(eng, out, in0, in1, op, r, w):
        S.op(eng, lambda e: e.tensor_tensor(out=out, in0=in0, in1=in1, op=op), r, w)

    def ts(eng, out, in0, s1, s2, op0, op1, r, w):
        if s2 is None:
            S.op(eng, lambda e: e.tensor_scalar(out=out, in0=in0, scalar1=s1, scalar2=None, op0=op0), r, w)
        else:
            S.op(eng, lambda e: e.tensor_scalar(out=out, in0=in0, scalar1=s1, scalar2=s2, op0=op0, op1=op1), r, w)

    def stt(eng, out, in0, scalar, in1, op0, op1, r, w):
        S.op(eng, lambda e: e.scalar_tensor_tensor(out=out, in0=in0, scalar=scalar, in1=in1, op0=op0, op1=op1), r, w)

    def cp(eng, out, in_, r, w):
        if eng == 'act':
            act(out, in_, AF.Copy, r, w)
        else:
            S.op(eng, lambda e: e.tensor_copy(out=out, in_=in_), r, w)

    def rmax(out, in_, r, w):
        S.op('dve', lambda e: e.reduce_max(out=out, in_=in_, axis=AX), r, w)

    def dma(eng, out, in_, key, r, w, final=False):
        S.op(eng, lambda e: e.dma_start(out=out, in_=in_), r, w, dma=key, final=final)

    evac_rr = [0]

    def evac_eng():
        evac_rr[0] += 1
        ev = os.environ.get("K_EV", "")
        if ev:
            return ev
        return 'act' if evac_rr[0] % 2 else 'dve'

    def scaled_copy(eng, out, in_, scale, r, w):
        if eng == 'act':
            act(out, in_, AF.Copy, r, w, scale=scale)
        else:
            ts(eng, out, in_, scale, None, ALU.mult, None, r, w)

    cst = sbt(top, "cst", [128, C_DM], F32)
    cstb = sbt(top, "cstb", [128, 384 + 2048], BF16)
    par = sbt(top, "par", [128, NPAR], F32)
    bsbx = sbt(top, "bsbx", [128, 8], F32)
    mixT = sbt(top, "mixT", [128, 16, 1028], BF16)

    IDf = cst[:, C_ID:C_ID + 128]; TLE = cst[:, C_TLE:C_TLE + 128]; ONE = cst[:, C_ONE:C_ONE + 128]
    CM = cst[:, C_CM:C_CM + 128]; NTGf = cst[:, C_NTG:C_NTG + 128]; NONf = cst[:, C_NON:C_NON + 128]
    NM = cst[:, C_NM:C_NM + 32]
    IDb = cstb[:, 0:128]; NTGb = cstb[:, 128:256]; NONb = cstb[:, 256:384]

    def DMb(r):
        return cstb[:, 384 + r * 512: 384 + (r + 1) * 512]

    dma('sp', cst[:, :], cst_d[:, 0:C_DM], 'c0', [], ['cst'])
    dma('sp', par[:, :], par_d[:, :], 'c1', [], ['par'])
    dma('pool', cstb[:, 0:128], cst_d[:, C_ID:C_ID + 128], 'c2', [], ['cstb'])
    dma('pool', cstb[:, 128:384], cst_d[:, C_NTG:C_NTG + 256], 'c2', [], ['cstb'])
    dma('pool', cstb[:, 384:384 + 2048], cst_d[:, C_DM:C_DM + 2048], 'c2', [], ['cstb'])
    ts('dve', bsbx[:, :], par[:, P_BSB:P_BSB + 8], par[:, P_CTX:P_CTX + 1], None, ALU.add, None, ['par'], ['bsbx'])

    def tile_info(ti):
        return (ti * 128, 128) if ti < 16 else (2048, 4)

    def mixcol(ti):
        return (ti - 8) * 128 if ti < 16 else 1024

    NWS = 4
    wctr = [0]

    def load_w(ws, src_ap, ncols, shape3=None):
        si = wctr[0] % NWS
        wctr[0] += 1
        slot = ws[si]
        key = ('w', si)
        return slot, key, si

    L0 = ExitStack()
    hT = sbt(L0, "hT", [128, 16, 2052], BF16)
    wsl = [sbt(L0, "ws%d" % i, [128, 16, 256], BF16) for i in range(NWS)]

    W0LIST = []
    for h_ in range(4):
        W0LIST += [1024 + h_ * 256, 2048 + h_ * 256, 0 + h_ * 256, 3072 + h_ * 256, 4096 + h_ * 256]
    for hp_ in range(4):
        W0LIST += [5128 + hp_ * 256, 6152 + hp_ * 256, 7176 + hp_ * 256, 8200 + hp_ * 256]
    w0i = [0, 0]

    def w0_issue_upto(n):
        while w0i[0] < min(n, len(W0LIST)):
            i = w0i[0]
            si = i % NWS
            slot = wsl[si]
            src = w0cols(W0LIST[i], 256)
            S.op('pool', lambda e, slot=slot, src=src: e.dma_start(out=slot[:, :, :], in_=src), [], [('w', si)], dma=('w', si))
            w0i[0] += 1

    def wget(c0, prefetch=0):
        i = w0i[1]
        assert W0LIST[i] == c0, (i, W0LIST[i], c0)
        w0i[1] += 1
        w0_issue_upto(i + 1 + prefetch)
        return wsl[i % NWS], ('w', i % NWS)

    def w0cols(c0, ncols):
        return w_in0[:, c0:c0 + ncols].rearrange("(k p) j -> p k j", p=128)

    NT1 = int(os.environ.get('K_NT1', '17')) if stage >= 1 else 0
    with ExitStack() as P1:
        xt = [sbt(P1, "xt%d" % i, [128, 2048], F32) for i in range(2)]
        xn = [sbt(P1, "xn%d" % i, [128, 2048], F32) for i in range(2)]
        junk = sbt(P1, "junk", [128, 2048], BF16)
        st = sbt(P1, "st1", [128, 8], F32)
        ptr = [pst(P1, "ptr%d" % i, [128, 512], F32) for i in range(4)]
        for ti in range(NT1):
            tok0, L = tile_info(ti)
            sl = ti % 2
            src = xw[tok0:tok0 + 128, :] if ti < 16 else xs[:, :]
            PL = int(os.environ.get("K_P1", "9"))
            dma('sp', xt[sl][:L, :], src, ('xt', sl), [], [('xt', sl)])
            if PL >= 1:
                act(junk[:L, :], xt[sl][:L, :], AF.Square, [('xt', sl)], ['junk', 'ss'], accum=st[:L, 0:1])
            if PL >= 2:
                ts('dve', st[:L, 1:2], st[:L, 0:1], 1.0 / 2048, EPS, ALU.mult, ALU.add, ['ss'], ['ms'])
                act(st[:L, 2:3], st[:L, 1:2], AF.Ln, ['ms'], ['ln'])
                act(st[:L, 3:4], st[:L, 2:3], AF.Exp, ['ln'], ['rstd'], scale=-0.5)
            if PL >= 3:
                ts('dve', xn[sl][:L, :], xt[sl][:L, :], st[:L, 3:4], None, ALU.mult, None, [('xt', sl), 'rstd'], [('xn', sl)])
            for half in range(4):
                for j in range(4):
                    kc = half * 4 + j
                    if PL >= 4:
                        tr(ptr[half][:, j * 128:j * 128 + L], xn[sl][:L, kc * 128:(kc + 1) * 128], IDf[:L, :L],
                           [('xn', sl), 'cst'], [('ptr', half, j)])
                if PL >= 5 and os.environ.get("K_T") == "A":
                    act(junk[:, 0:512], ptr[half][:, :], AF.Copy, [('ptr', half, j) for j in range(4)], ['junk'])
                elif PL >= 5 and os.environ.get("K_T") == "B":
                    for j in range(4):
                        act(junk[:, j * 128:(j + 1) * 128], ptr[half][:, j * 128:(j + 1) * 128], AF.Copy, [('ptr', half, j)], ['junk'])
                elif PL >= 5 and os.environ.get("K_T") == "C":
                    act(junk[:, 0:512], ptr[half][:, :], AF.Copy, [('ptr', half, j) for j in range(4)] + ['par'], ['junk'], scale=par[:, 0:1])
                elif PL >= 5:
                    for j in range(4):
                        kc = half * 4 + j
                        scaled_copy(evac_eng(), hT[:, kc, tok0:tok0 + L], ptr[half][:, j * 128:j * 128 + L],
                                    par[:, P_G0 + kc:P_G0 + kc + 1], [('ptr', half, j), 'par'], [('hT', ti)])
    S.barrier()

    def hT_reads(t0, n):
        tiles = set()
        for t in range(t0, t0 + n, 4):
            tiles.add(min(t // 128, 16))
        tiles.add(min((t0 + n - 1) // 128, 16))
        return [('hT', t) for t in sorted(tiles)]

    def proj_fm(ps, pkey, slot, wkey, c0, t0, n):
        rd = hT_reads(t0, n) + [wkey]
        for kc in range(16):
            mm(ps[:, 0:n], slot[:, kc, c0:c0 + 128], hT[:, kc, t0:t0 + n], kc == 0, kc == 15, rd, [pkey])

    def proj_tm(ps, pkey, slot, wkey, c0, ncols, ti):
        tok0, L = tile_info(ti)
        rd = [('hT', ti), wkey]
        for kc in range(16):
            mm(ps[:L, 0:ncols], hT[:, kc, tok0:tok0 + L], slot[:, kc, c0:c0 + ncols], kc == 0, kc == 15, rd, [pkey])

    if stage >= 2:
        with ExitStack() as SA:
            wg = sbt(SA, "wg", [128, 16, 8], BF16)
            IG = sbt(SA, "IG", [128, 17, 4], F32); LF = sbt(SA, "LF", [128, 17, 4], F32)
            Aa = sbt(SA, "Aa", [128, 17, 4], F32); Bc = sbt(SA, "Bc", [128, 17, 4], F32)
            BT = sbt(SA, "BT", [128, 17, 4], F32)
            gt = sbt(SA, "gt", [128, 16], F32)
            qT = sbt(SA, "qT", [128, 2, 1028], BF16); kT = sbt(SA, "kT", [128, 2, 1028], BF16)
            kk = sbt(SA, "kk", [128, 17, 256], BF16); vv = sbt(SA, "vv", [128, 17, 257], BF16)
            og = sbt(SA, "og", [128, 9, 256], F32)
            sgt = sbt(SA, "sgt", [128, 512], F32)
            CT = sbt(SA, "CT", [128, 2, 257], F32); CTb = sbt(SA, "CTb", [128, 2, 257], BF16)
            m0 = sbt(SA, "m0", [128, 1], F32)
            dg = sbt(SA, "dg", [128, 128], F32); Am = sbt(SA, "Am", [128, 128], F32)
            Dm = sbt(SA, "Dm", [128, 128], F32); Sw = sbt(SA, "Sw", [128, 128], BF16)
            SwT = sbt(SA, "SwT", [128, 128], BF16)
            sc = sbt(SA, "sc", [128, 16], F32)
            tmpi = sbt(SA, "tmpi", [128, 256], F32); hh = sbt(SA, "hh", [128, 256], F32)
            mixa = sbt(SA, "mixa", [128, 256], BF16); vw = sbt(SA, "vw", [128, 257], BF16)
            pm = pst(SA, "pm", [128, 512], F32)
            pin = pst(SA, "pin", [128, 512], F32); pit = pst(SA, "pit", [128, 512], F32)
            pu = [pst(SA, "pu%d" % i, [128, 512], F32) for i in range(2)]
            pj = [pst(SA, "pj%d" % i, [128, 512], F32) for i in range(2)]
            ptb = pst(SA, "ptb", [128, 1024], BF16)

            S.op('pool', lambda e: e.dma_start(out=wg[:, :, :], in_=w0cols(5120, 8)), [], ['wg'], dma='wg')
            for ti in range(17):
                tok0, L = tile_info(ti)
                for kc in range(16):
                    mm(pm[:L, 256:264], hT[:, kc, tok0:tok0 + L], wg[:, kc, :], kc == 0, kc == 15, [('hT', ti), 'wg'], ['pm_g'])
                tt('dve', IG[:L, ti, :], pm[:L, 256:260], par[:L, P_BI:P_BI + 4], ALU.add, ['pm_g', 'par'], [('IG', ti)])
                tt('dve', gt[:L, 0:4], pm[:L, 260:264], par[:L, P_BF:P_BF + 4], ALU.add, ['pm_g', 'par'], ['gt0'])
                act(gt[:L, 4:8], gt[:L, 0:4], AF.Exp, ['gt0'], ['gt1'], scale=-1.0)
                act(gt[:L, 8:12], gt[:L, 4:8], AF.Ln, ['gt1'], ['gt2'], bias=1.0)
                ts('dve', LF[:L, ti, :], gt[:L, 8:12], -1.0, None, ALU.mult, None, ['gt2'], [('LF', ti)])
                mm(pm[:L, 268:272], TLE[:L, :L], LF[:L, ti, :], True, True, [('LF', ti), 'cst'], ['pm_b'])
                mm(pm[:, 264:268], ONE[:L, :], LF[:L, ti, :], True, True, [('LF', ti), 'cst'], ['pm_bt'])
                tt('dve', Aa[:L, ti, :], IG[:L, ti, :], pm[:L, 268:272], ALU.subtract, [('IG', ti), 'pm_b'], [('Aa', ti)])
                cp('dve', Bc[:L, ti, :], pm[:L, 268:272], ['pm_b'], [('Bc', ti)])
                cp('dve', BT[:, ti, :], pm[:, 264:268], ['pm_bt'], [('BT', ti)])

            pjr = [0]

            def nextpj():
                pjr[0] += 1
                i = pjr[0] % 2
                return pj[i], ('pj', i)

            from collections import deque
            Q = deque()
            credit = [0.0, 0.0]

            def pump():
                credit[0] += credit[1]
                while credit[0] >= 1.0 and Q:
                    credit[0] -= 1.0
                    Q.popleft()()

            def flush():
                while Q:
                    Q.popleft()()
                credit[0] = 0.0

            S.op('pool', lambda e: e.memset(vv[:, :, 256:257], 1.0), [], ['vv'])

            def items_for(h):
                W = {}
                base = {'k': 1024, 'v': 2048, 'q': 0, 'o': 3072, 'g': 4096}

                def need(name, prefetch=0):
                    if name not in W:
                        W[name] = wget(base[name] + h * 256, prefetch=prefetch)
                    return W[name]

                def kv(ti):
                    def f():
                        wk_, wkk = need('k')
                        wv, wvk = need('v')
                        tok0, L = tile_info(ti)
                        ps, pk = nextpj()
                        proj_tm(ps, pk, wk_, wkk, 0, 256, ti)
                        scaled_copy(evac_eng(), kk[:L, ti, :], ps[:L, 0:256], 1.0 / 16, [pk], [('kk', ti)])
                        ps, pk = nextpj()
                        proj_tm(ps, pk, wv, wvk, 0, 256, ti)
                        cp(evac_eng(), vv[:L, ti, 0:256], ps[:L, 0:256], [pk, 'vv'], [('vv', ti)])
                    return f

                def fm(c2, t0, n, mc, which):
                    def f():
                        ps, pk = nextpj()
                        if which == 'q':
                            wq, wqk = need('q', prefetch=1)
                            proj_fm(ps, pk, wq, wqk, c2 * 128, t0, n)
                            cp(evac_eng(), qT[:, c2, mc:mc + n], ps[:, 0:n], [pk], ['qT'])
                        else:
                            wk_, wkk = need('k')
                            proj_fm(ps, pk, wk_, wkk, c2 * 128, t0, n)
                            scaled_copy(evac_eng(), kT[:, c2, mc:mc + n], ps[:, 0:n], 1.0 / 16, [pk], ['kT'])
                    return f

                def ogi(ti):
                    def f():
                        wo, wok = need('o')
                        wgg, wggk = need('g')
                        tok0, L = tile_info(ti)
                        ps, pk = nextpj()
                        proj_tm(ps, pk, wo, wok, 0, 256, ti)
                        ps2, pk2 = nextpj()
                        proj_tm(ps2, pk2, wgg, wggk, 0, 256, ti)
                        act(sgt[:L, 0:256], ps[:L, 0:256], AF.Exp, [pk], ['sgt0'], scale=-1.0)
                        act(sgt[:L, 256:512], ps2[:L, 0:256], AF.Exp, [pk2], ['sgt1'], scale=-1.0)
                        act(sgt[:L, :], sgt[:L, :], AF.Ln, ['sgt0', 'sgt1'], ['sgt0', 'sgt1'], bias=1.0)
                        tt('dve', sgt[:L, 0:256], sgt[:L, 0:256], sgt[:L, 256:512], ALU.add, ['sgt0', 'sgt1'], ['sgt0'])
                        act(sgt[:L, 0:256], sgt[:L, 0:256], AF.Exp, ['sgt0'], ['sgt0'], scale=-1.0)
                        tt('dve', og[:L, ti - 8, :], sgt[:L, 0:256], ps2[:L, 0:256], ALU.mult, ['sgt0', pk2], [('og', ti)])
                    return f

                fms = []
                for c2 in range(2):
                    for (t0, n, mc) in ((1024, 512, 0), (1536, 512, 512), (2048, 4, 1024)):
                        fms.append(fm(c2, t0, n, mc, 'q'))
                        fms.append(fm(c2, t0, n, mc, 'k'))
                return kv, fms, ogi

            def step(h, ti):
                tok0, L = tile_info(ti)
                full = ti >= 8
                if ti == 16:
                    for kc in range(2):
                        dma('sp', CT[:, kc, :], aCT_d[h, kc * 128:(kc + 1) * 128, :], 'st_in', [], ['CT'])
                    cp('dve', m0[:, :], par[:, P_AM + h:P_AM + h + 1], ['par'], ['m0'])
                    cp('act', CTb[:, :, :], CT[:, :, :], ['CT'], ['CTb'])
                a_col = Aa[:L, ti, h:h + 1]
                ts('dve', dg[:L, :L], IDf[:L, :L], a_col, None, ALU.mult, None, [('Aa', ti), 'cst'], ['dg'])
                mm(pm[:, 0:L], ONE[:L, :], dg[:L, :L], True, True, ['dg', 'cst'], ['pm_a'])
                pump()
                rmax(sc[:, 0:1], pm[:, 0:L], ['pm_a'], ['sc0'])
                ts('dve', sc[:, 1:2], sc[:, 0:1], m0[:, 0:1], -1.0, ALU.max, ALU.mult, ['sc0', 'm0'], ['nml'])
                if full:
                    mc = mixcol(ti)
                    tt('dve', Am[:L, :L], pm[:L, 0:L], CM[:L, :L], ALU.add, ['pm_a', 'cst'], ['Am'])
                    rmax(sc[:L, 2:3], Am[:L, :L], ['Am'], ['sc2'])
                    ts('dve', sc[:L, 3:4], sc[:L, 2:3], m0[:L, 0:1], -1.0, ALU.max, ALU.mult, ['sc2', 'm0'], ['negM'])
                    act(Dm[:L, :L], Am[:L, :L], AF.Exp, ['Am', 'negM'], ['Dm'], bias=sc[:L, 3:4])
                    for kc in range(2):
                        mm(pm[:L, 128:128 + L], qT[:, kc, mc:mc + L], kT[:, kc, mc:mc + L], kc == 0, kc == 1, ['qT', 'kT'], ['pm_s'])
                    pump()
                    tt('dve', Sw[:L, :L], pm[:L, 128:128 + L], Dm[:L, :L], ALU.mult, ['pm_s', 'Dm'], ['Sw'])
                    tr(ptb[:L, 0:L], Sw[:L, :L], IDb[:L, :L], ['Sw', 'cstb'], ['ptb0'])
                    pump()
                    cp('act', SwT[:L, :L], ptb[:L, 0:L], ['ptb0'], ['SwT'])
                    mm(pin[:L, 0:257], SwT[:L, :L], vv[:L, ti, :], True, True, ['SwT', ('vv', ti)], ['pin'])
                    for kc in range(2):
                        mm(pit[:L, 0:257], qT[:, kc, mc:mc + L], CTb[:, kc, :], kc == 0, kc == 1, ['qT', 'CTb'], ['pit'])
                    act(sc[:L, 4:5], sc[:L, 3:4], AF.Exp, ['negM', 'm0'], ['wc'], bias=m0[:L, 0:1])
                    pump()
                    cp('dve', sc[:L, 5:6], pin[:L, 256:257], ['pin'], ['c1'])
                    stt('dve', sc[:L, 6:7], pit[:L, 256:257], sc[:L, 4:5], sc[:L, 5:6], ALU.mult, ALU.add, ['pit', 'wc', 'c1'], ['den'])
                    act(sc[:L, 7:8], sc[:L, 6:7], AF.Abs, ['den'], ['aden'])
                    act(sc[:L, 8:9], Bc[:L, ti, h:h + 1], AF.Exp, [('Bc', ti), 'negM'], ['lowb'], bias=sc[:L, 3:4], scale=-1.0)
                    tt('dve', sc[:L, 9:10], sc[:L, 7:8], sc[:L, 8:9], ALU.max, ['aden', 'lowb'], ['dmax'])
                    S.op('dve', lambda e, L=L: e.reciprocal(out=sc[:L, 10:11], in_=sc[:L, 9:10]), ['dmax'], ['rden'])
                    tt('dve', sc[:L, 11:12], sc[:L, 10:11], sc[:L, 4:5], ALU.mult, ['rden', 'wc'], ['wr'])
                    act(tmpi[:L, :], pit[:L, 0:256], AF.Copy, ['pit', 'wr'], ['tmpi'], scale=sc[:L, 11:12])
                    stt('dve', hh[:L, :], pin[:L, 0:256], sc[:L, 10:11], tmpi[:L, :], ALU.mult, ALU.add, ['pin', 'rden', 'tmpi'], ['hh'])
                    tt('dve', mixa[:L, :], hh[:L, :], og[:L, ti - 8, :], ALU.mult, ['hh', ('og', ti)], ['mixa'])
                    pump()
                    for j in range(2):
                        tr(ptb[:, 256 + j * 128:256 + j * 128 + L], mixa[:L, j * 128:(j + 1) * 128], IDb[:L, :L], ['mixa', 'cstb'], [('ptb1', j)])
                        cp(evac_eng(), mixT[:, h * 2 + j, mc:mc + L], ptb[:, 256 + j * 128:256 + j * 128 + L], [('ptb1', j)], ['mixT'])
                act(sc[:L, 12:13], a_col, AF.Exp, [('Aa', ti), 'nml'], ['wl'], bias=sc[:L, 1:2])
                act(sc[:, 13:14], sc[:, 1:2], AF.Exp, ['nml', 'm0'], ['wcl'], bias=m0[:, 0:1])
                ts('dve', vw[:L, :], vv[:L, ti, :], sc[:L, 12:13], None, ALU.mult, None, [('vv', ti), 'wl'], ['vw'])
                pump()
                for kc in range(2):
                    mm(pu[kc][:, 0:257], kk[:L, ti, kc * 128:(kc + 1) * 128], vw[:L, :], True, True, [('kk', ti), 'vw'], [('pu', kc)])
                    stt('dve', CT[:, kc, :], CT[:, kc, :], sc[:, 13:14], pu[kc][:, 0:257], ALU.mult, ALU.add, ['CT', 'wcl', ('pu', kc)], ['CT'])
                tt('dve', m0[:, :], BT[:, ti, h:h + 1], sc[:, 1:2], ALU.subtract, [('BT', ti), 'nml'], ['m0'])
                pump()
                if ti == 7:
                    ts('dve', CT[:, :, :], CT[:, :, :], par[:, P_FLAG:P_FLAG + 1], None, ALU.mult, None, ['CT', 'par'], ['CT'])
                    ts('dve', m0[:, :], m0[:, :], par[:, P_FLAG:P_FLAG + 1], None, ALU.mult, None, ['m0', 'par'], ['m0'])
                if ti < 15:
                    cp('act', CTb[:, :, :], CT[:, :, :], ['CT'], ['CTb'])
                if ti == 15 or ti == 16:
                    dst = CTo_p if ti == 15 else CTo_s
                    mdst = mo_p if ti == 15 else mo_s
                    for kc in range(2):
                        dma('sp', dst[h, kc * 128:(kc + 1) * 128, :], CT[:, kc, :], 'st_out', ['CT'], [], final=True)
                    dma('sp', mdst[0:1, h:h + 1], m0[0:1, 0:1], 'st_out', ['m0'], [], final=True)

            ITEMS = [items_for(h) for h in range(4)]
            for t in range(8):
                ITEMS[0][0](t)()
            for h in range(4):
                kv, fms, ogi = ITEMS[h]
                ctxq = fms + [kv(t) for t in range(8, 17)] + [ogi(8)]
                Q.extend(ctxq)
                credit[0] = 0.0
                credit[1] = len(ctxq) / (8 * 3.0) + 0.01
                S.op('pool', lambda e: e.memset(CT[:, :, :], 0.0), [], ['CT'])
                S.op('pool', lambda e: e.memset(CTb[:, :, :], 0.0), [], ['CTb'])
                S.op('pool', lambda e: e.memset(m0[:, :], 0.0), [], ['m0'])
                for ti in range(8):
                    step(h, ti)
                flush()
                for ti in range(8, 17):
                    add = []
                    if ti < 16:
                        add.append(ogi(ti + 1))
                        if h < 3:
                            add.append(ITEMS[h + 1][0](ti - 8))
                    Q.extend(add)
                    credit[0] = 0.0
                    credit[1] = len(add) / 6.0
                    step(h, ti)
                    flush()
            w0_issue_upto(w0i[1] + 4)
        S.barrier()

    if stage >= 3:
        with ExitStack() as SB:
            qTb = sbt(SB, "qTb", [128, 2, 1028], BF16); kTb = sbt(SB, "kTb", [128, 2, 2052], BF16)
            vb = sbt(SB, "vb", [128, 17, 256], BF16); sgT = sbt(SB, "sgT", [128, 2, 1028], BF16)
            stg = [sbt(SB, "stg%d" % i, [128, 256], F32) for i in range(4)]
            gtmp = sbt(SB, "gtmp", [128, 512], F32)
            qTs = sbt(SB, "qTs", [128, 8, 4], BF16); kTs = sbt(SB, "kTs", [128, 8, 4], BF16)
            vs = sbt(SB, "vs", [4, 1024], BF16)
            gsT = sbt(SB, "gsT", [128, 8, 4], BF16)
            pj = [pst(SB, "pjb%d" % i, [128, 512], F32) for i in range(2)]
            ATT = ExitStack()
            ee = [sbt(ATT, "ee%d" % g, [128, 512], F32) for g in range(2)]
            Lb = [[sbt(ATT, "Lb%d_%d" % (g, i), [128, 512], BF16) for i in range(2)] for g in range(2)]
            aa = [[sbt(ATT, "aa%d_%d" % (g, i), [128, 512], BF16) for i in range(2)] for g in range(2)]
            Acc = [sbt(ATT, "Acc%d" % g, [128, 512], F32) for g in range(2)]
            Accb = [[sbt(ATT, "Accb%d_%d" % (g, i), [128, 512], BF16) for i in range(2)] for g in range(2)]
            pP = [[pst(ATT, "pP%d_%d" % (g, i), [128, 512], F32) for i in range(2)] for g in range(2)]
            pO = [pst(ATT, "pO%d" % g, [128, 512], F32) for g in range(2)]
            pjr = [0]

            def nextpj():
                pjr[0] += 1
                i = pjr[0] % 2
                return pj[i], ('pj', i)

            stgr = [0]

            def nextstg():
                stgr[0] += 1
                i = stgr[0] % 4
                return stg[i], ('stg', i)

            SCL = float(128 ** -0.5)
            ucount = [0]
            for hp in range(4):
                wq, wqk = wget(5128 + hp * 256)
                wk_, wkk = wget(6152 + hp * 256)
                wv, wvk = wget(7176 + hp * 256)
                wgb, wgbk = wget(8200 + hp * 256)
                for hh_ in range(2):
                    for (t0, n, mc) in ((1024, 512, 0), (1536, 512, 512), (2048, 4, 1024)):
                        ps, pk = nextpj()
                        proj_fm(ps, pk, wq, wqk, hh_ * 128, t0, n)
                        scaled_copy(evac_eng(), qTb[:, hh_, mc:mc + n], ps[:, 0:n], SCL, [pk], ['qTb'])
                        ps, pk = nextpj()
                        proj_fm(ps, pk, wgb, wgbk, hh_ * 128, t0, n)
                        act(gtmp[:, 0:n], ps[:, 0:n], AF.Exp, [pk], ['gtmp'], scale=-1.0)
                        act(gtmp[:, 0:n], gtmp[:, 0:n], AF.Ln, ['gtmp'], ['gtmp'], bias=1.0)
                        act(gtmp[:, 0:n], gtmp[:, 0:n], AF.Exp, ['gtmp'], ['gtmp'], scale=-1.0)
                        tt('dve', sgT[:, hh_, mc:mc + n], gtmp[:, 0:n], ps[:, 0:n], ALU.mult, ['gtmp', pk], ['sgT'])
                    for (t0, n) in ((0, 512), (512, 512), (1024, 512), (1536, 512), (2048, 4)):
                        ps, pk = nextpj()
                        proj_fm(ps, pk, wk_, wkk, hh_ * 128, t0, n)
                        cp(evac_eng(), kTb[:, hh_, t0:t0 + n], ps[:, 0:n], [pk], ['kTb'])
                    cp('dve', qTs[:, hp * 2 + hh_, :], qTb[:, hh_, 1024:1028], ['qTb'], ['qTs'])
                    cp('dve', kTs[:, hp * 2 + hh_, :], kTb[:, hh_, 2048:2052], ['kTb'], ['kTs'])
                    cp('dve', gsT[:, hp * 2 + hh_, :], sgT[:, hh_, 1024:1028], ['sgT'], ['gsT'])
                for ti in range(17):
                    tok0, L = tile_info(ti)
                    ps, pk = nextpj()
                    proj_tm(ps, pk, wv, wvk, 0, 256, ti)
                    cp('act', vb[:L, ti, :], ps[:L, 0:256], [pk], [('vb', ti)])
                    if ti >= 8:
                        sg_, sgk = nextstg()
                        cp('dve', sg_[:L, :], ps[:L, 0:256], [pk], [sgk])
                        if ti < 16:
                            dma('sp', bv_p[(ti - 8) * 128:(ti - 7) * 128, hp * 256:(hp + 1) * 256], sg_[:, :], sgk, [sgk], [], final=True)
                        else:
                            dma('sp', bv_s[:, hp * 256:(hp + 1) * 256], sg_[:4, :], sgk, [sgk], [], final=True)
                            cp('dve', vs[:4, hp * 256:(hp + 1) * 256], ps[:4, 0:256], [pk], ['vs'])
                        ps, pk = nextpj()
                        proj_tm(ps, pk, wk_, wkk, 0, 256, ti)
                        sg_, sgk = nextstg()
                        cp(evac_eng(), sg_[:L, :], ps[:L, 0:256], [pk], [sgk])
                        if ti < 16:
                            dma('sp', bk_p[(ti - 8) * 128:(ti - 7) * 128, hp * 256:(hp + 1) * 256], sg_[:, :], sgk, [sgk], [], final=True)
                        else:
                            dma('sp', bk_s[:, hp * 256:(hp + 1) * 256], sg_[:4, :], sgk, [sgk], [], final=True)
                w0_issue_upto(w0i[1] + 4)
                if stage >= 4:
                    for G in range(2):
                        blocks = [(8 + jb, jb - 4 * G) for jb in range(4 * G + 3, -1, -1)] + [(kt, None) for kt in range(7, -1, -1)]
                        nb = len(blocks)
                        U = [(s_, bi) for bi in range(nb) for s_ in range(2)]

                        def info(k):
                            s_, bi = U[k]
                            kt, r = blocks[bi]
                            own = kt >= 8
                            h = hp * 2 + s_
                            bias = par[:, P_BSB + h:P_BSB + h + 1] if own else bsbx[:, h:h + 1]
                            bkey = 'par' if own else 'bsbx'
                            return s_, bi, kt, r, own, bias, bkey, bi % 2

                        def a1(k):
                            s_, bi, kt, r, own, bias, bkey, u = info(k)
                            pk = ('pP', s_, u)
                            mm(pP[s_][u][:, :], kTb[:, s_, kt * 128:(kt + 1) * 128], qTb[:, s_, G * 512:(G + 1) * 512], True, False, ['kTb', 'qTb'], [pk])
                            if own and r >= 0:
                                mm(pP[s_][u][:, :], IDb, DMb(r), False, False, ['cstb'], [pk])

                        def a2(k):
                            s_, bi, kt, r, own, bias, bkey, u = info(k)
                            pk = ('pP', s_, u)
                            act(ee[s_][:, :], pP[s_][u][:, :], AF.Exp, [pk, bkey], [('ee', s_)], bias=bias)
                            act(Lb[s_][u][:, :], ee[s_][:, :], AF.Ln, [('ee', s_)], [('Lb', s_, u)], bias=1.0)
                            if bi < nb - 1:
                                if bi == 0:
                                    cp('dve', Accb[s_][1 - u][:, :], Lb[s_][u][:, :], [('Lb', s_, u)], [('Accb', s_, 1 - u)])
                                    cp('dve', Acc[s_][:, :], Lb[s_][u][:, :], [('Lb', s_, u)], [('Acc', s_)])
                                else:
                                    tt('dve', Accb[s_][1 - u][:, :], Acc[s_][:, :], Lb[s_][u][:, :], ALU.add, [('Acc', s_), ('Lb', s_, u)], [('Accb', s_, 1 - u)])
                                    tt('dve', Acc[s_][:, :], Acc[s_][:, :], Lb[s_][u][:, :], ALU.add, [('Acc', s_), ('Lb', s_, u)], [('Acc', s_)])

                        def a3(k):
                            s_, bi, kt, r, own, bias, bkey, u = info(k)
                            pk = ('pP', s_, u)
                            mm(pP[s_][u][:, :], NTGb, Lb[s_][u][:, :], False, bi == 0, [('Lb', s_, u), 'cstb'], [pk])
                            if bi > 0:
                                mm(pP[s_][u][:, :], NONb, Accb[s_][u][:, :], False, True, [('Accb', s_, u), 'cstb'], [pk])

                        def a4(k):
                            s_, bi, kt, r, own, bias, bkey, u = info(k)
                            act(aa[s_][u][:, :], pP[s_][u][:, :], AF.Exp, [('pP', s_, u), bkey], [('aa', s_, u)], bias=bias)

                        def a5(k):
                            s_, bi, kt, r, own, bias, bkey, u = info(k)
                            mm(pO[s_][:, :], vb[:, kt, s_ * 128:(s_ + 1) * 128], aa[s_][u][:, :], bi == 0, bi == nb - 1, [('aa', s_, u), ('vb', kt)], [('pO', s_)])

                        NU = len(U)
                        a1(0); a1(1); a2(0)
                        for k in range(NU):
                            if k + 2 < NU: a1(k + 2)
                            if k + 1 < NU: a2(k + 1)
                            a3(k)
                            a4(k)
                            if k >= 1: a5(k - 1)
                        a5(NU - 1)
                        for s_ in range(2):
                            h = hp * 2 + s_
                            tt('dve', mixT[:, 8 + h, G * 512:(G + 1) * 512], pO[s_][:, :], sgT[:, s_, G * 512:(G + 1) * 512], ALU.mult, [('pO', s_), 'sgT'], ['mixT'])
            S.barrier()
            ATT.close()
            S.barrier()
            if stage >= 5:
                with ExitStack() as SS:
                    NKS = 6
                    kpg = [sbt(SS, "kpg%d" % i, [128, 1024], BF16) for i in range(NKS)]
                    vpg = [sbt(SS, "vpg%d" % i, [128, 1024], BF16) for i in range(NKS)]
                    KTt = [sbt(SS, "KTt%d" % i, [128, 1024], BF16) for i in range(2)]
                    ptbi = sbt(SS, "ptbi", [128, 128], I32); idx = sbt(SS, "idx", [128, 128], I32)
                    zb = [sbt(SS, "zb%d" % i, [128, 32], F32) for i in range(2)]
                    es = [sbt(SS, "es%d" % i, [128, 32], F32) for i in range(2)]
                    Ls = [sbt(SS, "Ls%d" % i, [128, 32], F32) for i in range(2)]
                    rr = [sbt(SS, "rr%d" % i, [128, 32], F32) for i in range(2)]
                    asb = [sbt(SS, "asb%d" % i, [128, 32], BF16) for i in range(2)]
                    AccS = sbt(SS, "AccS", [128, 32], F32)
                    Os = sbt(SS, "Os", [4, 1024], F32)
                    sgs = sbt(SS, "sgs", [128, 32], F32)
                    pkt = [pst(SS, "pkt%d" % i, [128, 1024], BF16) for i in range(2)]
                    pzb = [pst(SS, "pzb%d" % i, [128, 512], F32) for i in range(2)]
                    posb = [pst(SS, "posb%d" % i, [128, 512], F32) for i in range(2)]
                    dma('sp', ptbi[:, :], ptb_d[:, :], 'ptb', [], ['ptbi'])
                    ts('dve', idx[:, :], ptbi[:, :], 128.0, par[:, P_PIDX:P_PIDX + 1], ALU.mult, ALU.add, ['ptbi', 'par'], ['idx'])
                    bsbs = par[:, P_BSBS:P_BSBS + 32]
                    S.op('pool', lambda e: e.memset(AccS[:, :], 0.0), [], ['AccS'])

                    NPG = 128
                    PF = NKS - 2

                    def issue(i):
                        j = NPG - 1 - i
                        sl = i % NKS
                        S.op('pool', lambda e: e.indirect_dma_start(out=kpg[sl][:, :], out_offset=None, in_=ck_d[:, :],
                                                                    in_offset=bass.IndirectOffsetOnAxis(ap=idx[:, j:j + 1], axis=0)),
                             ['idx'], [('kpg', sl)], dma=('kpg', sl))
                        S.op('pool', lambda e: e.indirect_dma_start(out=vpg[sl][:, :], out_offset=None, in_=cv_d[:, :],
                                                                    in_offset=bass.IndirectOffsetOnAxis(ap=idx[:, j:j + 1], axis=0)),
                             ['idx'], [('vpg', sl)], dma=('vpg', sl))

                    for i in range(PF):
                        issue(i)

                    def st_elem1(u, Lk, newblk):
                        pz = pzb[u]; pzk = ('pzb', u)
                        tt('dve', zb[u][:Lk, :], pz[:Lk, 0:32], bsbs[:Lk, :], ALU.add, [pzk, 'par'], [('zb', u)])
                        if newblk:
                            tt('dve', zb[u][:Lk, :], zb[u][:Lk, :], NM[:Lk, :], ALU.add, [('zb', u), 'cst'], [('zb', u)])
                        act(es[u][:Lk, :], zb[u][:Lk, :], AF.Exp, [('zb', u)], [('es', u)])
                        act(Ls[u][:Lk, :], es[u][:Lk, :], AF.Ln, [('es', u)], [('Ls', u)], bias=1.0)

                    def st_cum(u, Lk, first, last, newblk):
                        pz = pzb[u]; pzk = ('pzb', u)
                        mm(pz[:Lk, 32:64], NTGf[:Lk, :Lk], Ls[u][:Lk, :], True, first, [('Ls', u), 'cst'], [pzk])
                        if not first:
                            mm(pz[:Lk, 32:64], NONf[:, :Lk], AccS[:, :], False, True, ['AccS', 'cst'], [pzk])
                        tt('dve', rr[u][:Lk, :], pz[:Lk, 32:64], zb[u][:Lk, :], ALU.add, [pzk, ('zb', u)], [('rr', u)])
                        act(asb[u][:Lk, :], rr[u][:Lk, :], AF.Exp, [('rr', u)], [('asb', u)])
                        if newblk:
                            cp('dve', AccS[:Lk, :], Ls[u][:Lk, :], [('Ls', u)], ['AccS'])
                        elif not last:
                            tt('dve', AccS[:, :], AccS[:, :], Ls[u][:, :], ALU.add, ['AccS', ('Ls', u)], ['AccS'])

                    def st_av(u, Lk, vsrc, vkeys, first, last):
                        for h in range(8):
                            mm(posb[h // 4][:4, (h % 4) * 128:(h % 4 + 1) * 128], asb[u][:Lk, h * 4:(h + 1) * 4], vsrc[:Lk, h * 128:(h + 1) * 128],
                               first, last, [('asb', u)] + vkeys, [('posb', h // 4)])

                    for h in range(8):
                        mm(pzb[1][:4, h * 4:(h + 1) * 4], kTs[:, h, :], qTs[:, h, :], True, True, ['kTs', 'qTs'], [('pzb', 1)])
                    st_elem1(1, 4, True)
                    st_cum(1, 4, True, False, True)
                    st_av(1, 4, vs, ['vs'], True, False)

                    def st_tr(i):
                        sl = i % NKS; u = i % 2
                        for h in range(8):
                            tr(pkt[u][:, h * 128:(h + 1) * 128], kpg[sl][:, h * 128:(h + 1) * 128], IDb, [('kpg', sl), 'cstb'], [('pkt', u)])
                        cp(evac_eng(), KTt[u][:, :], pkt[u][:, :], [('pkt', u)], [('KTt', u)])

                    def st_z(i):
                        u = i % 2
                        for h in range(8):
                            mm(pzb[u][:, h * 4:(h + 1) * 4], KTt[u][:, h * 128:(h + 1) * 128], qTs[:, h, :], True, True, [('KTt', u), 'qTs'], [('pzb', u)])
                        st_elem1(u, 128, False)

                    st_tr(0); st_tr(1); st_z(0)
                    for i in range(NPG):
                        if i + 2 < NPG: st_tr(i + 2)
                        if i + 1 < NPG: st_z(i + 1)
                        st_cum(i % 2, 128, False, i == NPG - 1, False)
                        if i >= 1:
                            st_av((i - 1) % 2, 128, vpg[(i - 1) % NKS], [('vpg', (i - 1) % NKS)], False, False)
                        if i + PF < NPG: issue(i + PF)
                    st_av((NPG - 1) % 2, 128, vpg[(NPG - 1) % NKS], [('vpg', (NPG - 1) % NKS)], False, True)
                    for q in range(2):
                        cp('dve', Os[:4, q * 512:(q + 1) * 512], posb[q][:4, :], [('posb', q)], ['Os'])
                    for h in range(8):
                        tr(pzb[0][:, 64 + h * 4:64 + (h + 1) * 4], Os[:4, h * 128:(h + 1) * 128], IDf[:4, :4], ['Os', 'cst'], [('pzb', 0)])
                    cp('dve', sgs[:, :], pzb[0][:, 64:96], [('pzb', 0)], ['sgs'])
                    tt('dve', mixT[:, 8:16, 1024:1028], sgs[:, :].rearrange("p (h t) -> p h t", t=4), gsT[:, :, :], ALU.mult, ['sgs', 'gsT'], ['mixT'])
        S.barrier()
    L0.close()
    S.barrier()

    if stage >= 6:
        L1s = ExitStack()
        x1 = sbt(L1s, "x1", [128, 9, 2048], F32)
        NW2 = 3
        ws2 = [sbt(L1s, "w2s%d" % i, [128, 4096], BF16) for i in range(NW2)]
        w2c = [0]

        W2LIST = []
        for cb_ in range(8):
            W2LIST.append((w_out0[:, cb_ * 256:(cb_ + 1) * 256].rearrange("(k p) j -> p k j", p=128), 256))
        for H_ in range(2):
            for vb__ in range(8):
                W2LIST.append((w_in1[:, 2048 + vb__ * 256:2048 + (vb__ + 1) * 256].rearrange("(k p) j -> p k j", p=128), 256))
            for g_ in range(8):
                W2LIST.append((w_in1[:, g_ * 256:(g_ + 1) * 256].rearrange("(k p) j -> p k j", p=128), 256))
                W2LIST.append((w_in1[:, 4096 + g_ * 256:4096 + (g_ + 1) * 256].rearrange("(k p) j -> p k j", p=128), 256))
                W2LIST.append((w_out1[g_ * 256:(g_ + 1) * 256, :].rearrange("(k p) j -> p k j", p=128), 2048))
        w2i = [0, 0]

        def w2_issue_upto(n):
            while w2i[0] < min(n, len(W2LIST)):
                i = w2i[0]
                si = i % NW2
                src, j = W2LIST[i]
                view = ws2[si][:, :].rearrange("p (k j) -> p k j", j=j)
                S.op('pool', lambda e, view=view, src=src: e.dma_start(out=view, in_=src), [], [('w2', si)], dma=('w2', si))
                w2i[0] += 1

        def wload2(src_unused, j, prefetch=0):
            i = w2i[1]
            assert W2LIST[i][1] == j
            w2i[1] += 1
            w2_issue_upto(i + 1 + prefetch)
            return ws2[i % NW2][:, :].rearrange("p (k j) -> p k j", j=j), ('w2', i % NW2)

        def lt_info(lt):
            return (lt * 128, 128) if lt < 8 else (1024, 4)

        with ExitStack() as P4:
            xsl = [sbt(P4, "xsl%d" % i, [128, 9, 256], F32) for i in range(2)]
            pj4 = [pst(P4, "pj4%d" % i, [128, 512], F32) for i in range(2)]
            cnt4 = 0
            for cb in range(8):
                slot, wk4 = wload2(None, 256, prefetch=2)
                xi = cb % 2
                dma('sp', xsl[xi][:, 0:8, :], xw[1024:2048, cb * 256:(cb + 1) * 256].rearrange("(t p) j -> p t j", p=128), ('xsl', xi), [], [('xsl', xi)])
                dma('sp', xsl[xi][:4, 8, :], xs[:, cb * 256:(cb + 1) * 256], ('xsl', xi), [], [('xsl', xi)])
                for lt in range(9):
                    c0, L = lt_info(lt)
                    cnt4 += 1
                    ps = pj4[cnt4 % 2]; pk = ('pj4', cnt4 % 2)
                    for kc in range(16):
                        mm(ps[:L, 0:256], mixT[:, kc, c0:c0 + L], slot[:, kc, :], kc == 0, kc == 15, ['mixT', wk4], [pk])
                    tt('dve', x1[:L, lt, cb * 256:(cb + 1) * 256], ps[:L, 0:256], xsl[xi][:L, lt, :], ALU.add, [pk, ('xsl', xi)], [('x1', lt)])
        S.barrier()

        with ExitStack() as LL:
            gv = sbt(LL, "gv", [128, 5, 2048], BF16)
            wmTs = sbt(LL, "wmTs", [128, 5, 1024], BF16)
            wmT = sbt(LL, "wmT", [128, 1024], BF16)
            big8 = sbt(LL, "big8", [128, 2048], F32)
            gbc = sbt(LL, "gbc", [128, 2048], F32)
            Bs = sbt(LL, "Bs", [128, 1024], F32)
            gu = sbt(LL, "gu", [128, 516], F32); th = sbt(LL, "th", [128, 516], F32); ug = sbt(LL, "ug", [128, 516], F32)
            svt = sbt(LL, "svt", [128, 128], F32)
            y1T = sbt(LL, "y1T", [128, 2, 516], BF16)
            xnb = sbt(LL, "xnb", [128, 2048], BF16)
            st2 = sbt(LL, "st2", [128, 16], F32)
            rv = sbt(LL, "rv", [128, 8], F32)
            ptr2 = [pst(LL, "ptr2%d" % i, [128, 1024], BF16) for i in range(2)]
            pa = [pst(LL, "pa%d" % i, [128, 512], F32) for i in range(2)]
            pb = [pst(LL, "pb%d" % i, [128, 512], F32) for i in range(2)]
            pc = [pst(LL, "pc%d" % i, [128, 512], F32) for i in range(2)]
            h1T = mixT

            dma('sp', Bs[:, :], bsbc_d[:, :], 'bs', [], ['Bs'])
            dma('sp', gbc[:, :], vgbc_d[:, :], 'gbc', [], ['gbc'])
            dma('sp', big8[:, 0:1024], wsT_d.rearrange("p g t -> p (g t)"), 'big8', [], ['big8'])
            for g in range(8):
                tt('dve', wmT[:, g * 128:(g + 1) * 128], big8[:, g * 128:(g + 1) * 128], TLE, ALU.mult, ['big8', 'cst'], ['wmT'])

            def rstd_of(src_ap, L, col_out, rkeys, wkey):
                act(big8[:L, :], src_ap, AF.Square, rkeys + ['big8'], ['big8', 'st2a'], accum=st2[:L, 0:1])
                ts('dve', st2[:L, 1:2], st2[:L, 0:1], 1.0 / 2048, EPS, ALU.mult, ALU.add, ['st2a'], ['st2b'])
                act(st2[:L, 2:3], st2[:L, 1:2], AF.Ln, ['st2b'], ['st2c'])
                act(col_out, st2[:L, 2:3], AF.Exp, ['st2c'], [wkey], scale=-0.5)

            cA = [0]; cB = [0]; cC = [0]
            for H in range(2):
                lts = list(range(4)) if H == 0 else list(range(4, 9))

                def lcol(lt):
                    return (lt - 4 * H) * 128

                for lt in lts:
                    c0, L = lt_info(lt)
                    rstd_of(x1[:L, lt, :], L, st2[:L, 3:4], [('x1', lt)], 'st2d')
                    ts('dve', xnb[:L, :], x1[:L, lt, :], st2[:L, 3:4], None, ALU.mult, None, [('x1', lt), 'st2d'], ['xnb'])
                    for half in range(2):
                        for j in range(8):
                            kc = half * 8 + j
                            tr(ptr2[half][:, j * 128:j * 128 + L], xnb[:L, kc * 128:(kc + 1) * 128], IDb[:L, :L], ['xnb', 'cstb'], [('ptr2', half, j)])
                        for j in range(8):
                            kc = half * 8 + j
                            scaled_copy(evac_eng(), h1T[:, kc, lcol(lt):lcol(lt) + L], ptr2[half][:, j * 128:j * 128 + L],
                                        par[:, P_G1 + kc:P_G1 + kc + 1], [('ptr2', half, j), 'par'], ['mixT'])
                for vb_ in range(8):
                    slot, wk1 = wload2(None, 256, prefetch=2)
                    for lt in lts:
                        c0, L = lt_info(lt)
                        cA[0] += 1
                        ps = pa[cA[0] % 2]; pk = ('pa', cA[0] % 2)
                        for kc in range(16):
                            mm(ps[:L, 0:256], h1T[:, kc, lcol(lt):lcol(lt) + L], slot[:, kc, :], kc == 0, kc == 15, ['mixT', wk1], [pk])
                        act(gv[:L, lt - 4 * H, vb_ * 256:(vb_ + 1) * 256], ps[:L, 0:256], AF.Gelu, [pk], [('gv', lt - 4 * H)])
                for lt in lts:
                    c0, L = lt_info(lt)
                    l = lt - 4 * H
                    rstd_of(gv[:L, l, :], L, rv[:L, l:l + 1], [('gv', l)], ('rv', l))
                    ts('dve', wmTs[:L, l, :], wmT[:L, :], rv[:L, l:l + 1], None, ALU.mult, None, ['wmT', ('rv', l)], [('wmTs', l)])
                if H == 1:
                    stt('dve', big8[:4, :], gv[:4, 4, :], rv[:4, 4:5], gbc[:4, :], ALU.mult, ALU.mult, [('gv', 4), ('rv', 4), 'gbc', 'big8'], ['big8'])
                    dma('sp', cv_s[:, :], big8[:4, :], 'big8', ['big8'], [], final=True)
                ngrp = [(0, 512)] if H == 0 else [(0, 512), (512, 4)]
                for g in range(8):
                    wu, wuk = wload2(None, 256, prefetch=0)
                    wg2, wg2k = wload2(None, 256, prefetch=1)
                    for c2 in range(2):
                        chunk = g * 2 + c2
                        for (lc0, n) in ngrp:
                            cB[0] += 1
                            ps = pb[cB[0] % 2]; pk = ('pb', cB[0] % 2)
                            for kc in range(16):
                                mm(ps[:, 0:n], wu[:, kc, c2 * 128:(c2 + 1) * 128], h1T[:, kc, lc0:lc0 + n], kc == 0, kc == 15, ['mixT', wuk], [pk])
                            act(gu[:, lc0:lc0 + n], ps[:, 0:n], AF.Gelu, [pk], ['gu'])
                            cB[0] += 1
                            ps = pb[cB[0] % 2]; pk = ('pb', cB[0] % 2)
                            for kc in range(16):
                                mm(ps[:, 0:n], wg2[:, kc, c2 * 128:(c2 + 1) * 128], h1T[:, kc, lc0:lc0 + n], kc == 0, kc == 15, ['mixT', wg2k], [pk])
                            act(th[:, lc0:lc0 + n], ps[:, 0:n], AF.Tanh, [pk], ['th'], scale=0.5)
                            ts('dve', th[:, lc0:lc0 + n], th[:, lc0:lc0 + n], 0.5, 0.5, ALU.mult, ALU.add, ['th'], ['th'])
                            tt('dve', th[:, lc0:lc0 + n], th[:, lc0:lc0 + n], ps[:, 0:n], ALU.mult, ['th', pk], ['th'])
                            tt('dve', ug[:, lc0:lc0 + n], gu[:, lc0:lc0 + n], th[:, lc0:lc0 + n], ALU.mult, ['gu', 'th'], ['ug'])
                        for lt in lts:
                            c0, L = lt_info(lt)
                            l = lt - 4 * H
                            cC[0] += 1
                            ps = pc[cC[0] % 2]; pk = ('pc', cC[0] % 2)
                            mm(ps[:, 0:L], gv[:L, l, chunk * 128:(chunk + 1) * 128], wmTs[:L, l, g * 128:g * 128 + L], True, True, [('gv', l), ('wmTs', l)], [pk])
                            stt('dve', svt[:, 0:L], ps[:, 0:L], par[:, P_VG + chunk:P_VG + chunk + 1], Bs[:, g * 128:g * 128 + L], ALU.mult, ALU.add, [pk, 'par', 'Bs'], ['svt'])
                            tt('dve', y1T[:, c2, lcol(lt):lcol(lt) + L], svt[:, 0:L], ug[:, lcol(lt):lcol(lt) + L], ALU.mult, ['svt', 'ug'], ['y1T'])
                    wo1, wo1k = wload2(None, 2048, prefetch=2)
                    for lt in lts:
                        c0, L = lt_info(lt)
                        for cb in range(4):
                            cC[0] += 1
                            ps = pc[cC[0] % 2]; pk = ('pc', cC[0] % 2)
                            for c2 in range(2):
                                mm(ps[:L, :], y1T[:, c2, lcol(lt):lcol(lt) + L], wo1[:, c2, cb * 512:(cb + 1) * 512], c2 == 0, c2 == 1, ['y1T', wo1k], [pk])
                            tt('dve', x1[:L, lt, cb * 512:(cb + 1) * 512], x1[:L, lt, cb * 512:(cb + 1) * 512], ps[:L, :], ALU.add, [('x1', lt), pk], [('x1', lt)])
            dma('sp', gbc[:, :], gfbc_d[:, :], 'gbc', [], ['gbc'])
            for lt in range(9):
                c0, L = lt_info(lt)
                rstd_of(x1[:L, lt, :], L, st2[:L, 4:5], [('x1', lt)], 'st2e')
                stt('dve', big8[:L, :], x1[:L, lt, :], st2[:L, 4:5], gbc[:L, :], ALU.mult, ALU.mult, [('x1', lt), 'st2e', 'gbc', 'big8'], ['big8'])
                if lt < 8:
                    dma('sp', y_p[lt * 128:(lt + 1) * 128, :], big8[:, :], 'big8', ['big8'], [], final=True)
                else:
                    dma('sp', y_s[:, :], big8[:4, :], 'big8', ['big8'], [], final=True)
        S.barrier()
        L1s.close()

    S.finish()
    top.close()
    return nc


def _consts():
    c = np.zeros((128, NCST), np.float32)
    i = np.arange(128)
    c[:, C_ID:C_ID + 128] = np.eye(128)
    c[:, C_TLE:C_TLE + 128] = (i[:, None] <= i[None, :])
    c[:, C_ONE:C_ONE + 128] = 1.0
    c[:, C_CM:C_CM + 128] = np.where(i[None, :] <= i[:, None], 0.0, NEG)
    c[:, C_NTG:C_NTG + 128] = -(i[:, None] >= i[None, :]).astype(np.float32)
    c[:, C_NON:C_NON + 128] = -1.0
    nm = np.zeros((128, 32), np.float32)
    for s in range(4):
        for h in range(8):
            for t in range(4):
                nm[s, h * 4 + t] = 0.0 if s < t else NEG
    c[:, C_NM:C_NM + 32] = nm
    tq = np.arange(512)
    for r in range(4):
        c[:, C_DM + r * 512:C_DM + (r + 1) * 512] = np.where((r * 128 + i[:, None]) < tq[None, :], 0.0, NEG)
    return c


_CACHE = {}


def kernel(**inp):
    stage = int(os.environ.get("K_STAGE", "99"))
    f32 = np.float32
    xp = np.asarray(inp["x_prompt"], f32); xsm = np.asarray(inp["x_sample"], f32)
    ck = np.asarray(inp["cache_b_k"], f32)[0]; cv = np.asarray(inp["cache_b_v"], f32)[0]
    npool = ck.shape[0]
    ck2 = np.ascontiguousarray(ck.reshape(npool * 128, 1024)); cv2 = np.ascontiguousarray(cv.reshape(npool * 128, 1024))
    pt = np.asarray(inp["page_table"], np.int32)
    w_in0 = np.ascontiguousarray(inp["even_w_in"][0], f32); w_out0 = np.ascontiguousarray(inp["even_w_out"][0], f32)
    w_in1 = np.ascontiguousarray(inp["odd_w_in"][0], f32); w_out1 = np.ascontiguousarray(inp["odd_w_out"][0], f32)
    if os.environ.get("K_MINI", "0") == "1":
        w_in0 = w_in0[:, :16].copy(); w_out0 = w_out0[:, :16].copy(); w_in1 = w_in1[:, :16].copy(); w_out1 = w_out1[:, :16].copy()
    cst = _consts()
    key = (npool, stage)
    if key not in _CACHE:
        _CACHE[key] = build(npool, stage)
    nc = _CACHE[key]
    aC = np.asarray(inp["state_a_C"], f32)[0]; an = np.asarray(inp["state_a_n"], f32)[0]; am = np.asarray(inp["state_a_m"], f32)[0]
    in_maps = []
    NCR = int(os.environ.get("K_NCORES", "8"))
    for c in range(NCR):
        b, hf = c // 2, c % 2
        if hf == 1:
            xw = xp[b]
        else:
            xw = np.concatenate([np.zeros((1024, 2048), f32), xp[b, :1024]], axis=0)
        par = np.zeros((128, NPAR), f32)
        par[:, P_G0:P_G0 + 16] = inp["even_norm"][0].reshape(16, 128).T
        par[:, P_G1:P_G1 + 16] = inp["odd_norm"][0].reshape(16, 128).T
        par[:, P_VG:P_VG + 16] = inp["odd_v_gain"][0].reshape(16, 128).T
        par[:, P_BI:P_BI + 4] = inp["even_b_i"][0][None, :]
        par[:, P_BF:P_BF + 4] = inp["even_b_f"][0][None, :]
        par[:, P_BSB:P_BSB + 8] = inp["even_b_sb"][0][None, :]
        par[:, P_BSBS:P_BSBS + 32] = np.repeat(inp["even_b_sb"][0], 4)[None, :]
        par[:, P_FLAG] = float(hf)
        par[:, P_CTX] = 0.0 if hf == 1 else NEG
        par[:, P_AM:P_AM + 4] = am[c][None, :]
        par[:, P_PIDX] = np.arange(128)
        aCT = np.concatenate([aC[c].transpose(0, 2, 1), an[c][:, :, None]], axis=2)
        in_maps.append({
            "xw": np.ascontiguousarray(xw), "xs": np.ascontiguousarray(xsm[c]),
            "w_in0": w_in0, "w_out0": w_out0, "w_in1": w_in1, "w_out1": w_out1,
            "cst": cst, "par": par,
            "bsbc": np.ascontiguousarray(np.broadcast_to(inp["odd_b_s"][0].reshape(1, 1024), (128, 1024)), f32),
            "vgbc": np.ascontiguousarray(np.broadcast_to(inp["odd_v_gain"][0][None, :], (128, 2048)), f32),
            "gfbc": np.ascontiguousarray(np.broadcast_to(np.asarray(inp["final_norm"])[None, :], (128, 2048)), f32),
            "wsT": np.ascontiguousarray(np.asarray(inp["odd_w_s"][0], f32).transpose(2, 0, 1)),
            "aCT": np.ascontiguousarray(aCT, f32),
            "ck": ck2, "cv": cv2,
            "ptb": np.ascontiguousarray(np.broadcast_to(pt[c][None, :], (128, 128)), np.int32),
        })
    res = run_bass_kernel_spmd(nc, in_maps, core_ids=list(range(NCR)))
    R = list(res.results)
    while len(R) < 8:
        R.append({k: np.zeros_like(v) for k, v in R[0].items()})
    y_prompt = np.stack([np.concatenate([R[2 * b]["y_p"], R[2 * b + 1]["y_p"]], 0) for b in range(4)])
    y_sample = np.stack([R[c]["y_s"] for c in range(8)])
    aCp = np.stack([R[2 * b + 1]["CTo_p"][:, :, :256].transpose(0, 2, 1) for b in range(4)])[None]
    anp_ = np.stack([R[2 * b + 1]["CTo_p"][:, :, 256] for b in range(4)])[None]
    amp = np.stack([R[2 * b + 1]["mo_p"][0] for b in range(4)])[None]
    aCs = np.stack([R[c]["CTo_s"][:, :, :256].transpose(0, 2, 1) for c in range(8)])[None]
    ans = np.stack([R[c]["CTo_s"][:, :, 256] for c in range(8)])[None]
    ams = np.stack([R[c]["mo_s"][0] for c in range(8)])[None]
    bkp = np.stack([np.concatenate([R[2 * b]["bk_p"], R[2 * b + 1]["bk_p"]], 0) for b in range(4)]).reshape(1, 4, 2048, 8, 128)
    bvp = np.stack([np.concatenate([R[2 * b]["bv_p"], R[2 * b + 1]["bv_p"]], 0) for b in range(4)]).reshape(1, 4, 2048, 8, 128)
    bks = np.stack([R[c]["bk_s"] for c in range(8)]).reshape(1, 8, 4, 8, 128)
    bvs = np.stack([R[c]["bv_s"] for c in range(8)]).reshape(1, 8, 4, 8, 128)
    cvs = np.stack([R[c]["cv_s"] for c in range(8)])[None]
    outs = (y_prompt, y_sample, aCp, anp_, amp, aCs, ans, ams, bkp, bvp, bks, bvs, cvs)
    return tuple(np.ascontiguousarray(o, dtype=np.float32) for o in outs)
```

```python
import os
import numpy as np
import concourse.bass as bass
import concourse.mybir as mybir
from concourse.bass_utils import run_bass_kernel_spmd
from contextlib import ExitStack

F32 = mybir.dt.float32
BF16 = mybir.dt.bfloat16
I32 = mybir.dt.int32
AF = mybir.ActivationFunctionType
ALU = mybir.AluOpType
AX = mybir.AxisListType.X

NEG = -30000.0
EPS = 1e-6
C_ID, C_TLE, C_ONE, C_CM, C_NTG, C_NON, C_NM, C_DM = 0, 128, 256, 384, 512, 640, 768, 800
NCST = 800 + 2048
P_G0, P_G1, P_VG, P_BI, P_BF, P_BSB, P_BSBS, P_FLAG, P_CTX, P_AM, P_PIDX = 0, 16, 32, 48, 52, 56, 64, 96, 97, 98, 102
NPAR = 104

CENG = ('pe', 'act', 'dve', 'pool')


class Sched:
    def __init__(self, nc, stack):
        self.nc = nc
        self.eng = {'pe': nc.tensor, 'act': nc.scalar, 'dve': nc.vector, 'pool': nc.gpsimd, 'sp': nc.sync}
        self.sem = {e: stack.enter_context(nc.semaphore("sem_" + e)) for e in CENG}
        self.cnt = {e: 0 for e in CENG}
        self.known = {e: {} for e in self.eng}
        self.lastw = {}
        self.readers = {}
        self.dsem = {}
        self.dcnt = {}
        self.stack = stack
        self.final = []
        self.n_ops = 0

    def _wait(self, eng, tok):
        kind, f, val = tok
        if kind == 'E' and f == eng and eng == 'pe':
            return
        kk = (kind, f)
        if self.known[eng].get(kk, 0) >= val:
            return
        self.known[eng][kk] = val
        s = self.sem[f] if kind == 'E' else self.dsem[f]
        self.eng[eng].wait_ge(s, val)

    @staticmethod
    def _norm(k):
        if isinstance(k, str):
            if k.startswith('pm_'):
                return 'pm'
            if k in ('ptb0',):
                return 'ptb'
            if k in ('pz', 'pz2', 'pz3'):
                return 'pO'
            return k
        if k[0] in ('ptr', 'ptr2'):
            return (k[0], k[1])
        if k[0] == 'ptb1':
            return 'ptb'
        if k[0] == 'pos':
            return ('pP', k[1])
        return k

    @staticmethod
    def _is_psum(k):
        if isinstance(k, str):
            return k in ('pm', 'ptb', 'pO', 'pin', 'pit')
        return k[0] in ('ptr', 'ptr2', 'pP', 'pO', 'pzb', 'posb', 'pu', 'pj', 'pkt', 'pj4', 'pa', 'pb', 'pc')

    def op(self, eng, fn, reads=(), writes=(), dma=None, final=False):
        reads = [self._norm(k) for k in reads]
        writes = [self._norm(k) for k in writes]
        pr = [k for k in reads if self._is_psum(k)]
        if pr:
            writes = list(writes) + [k for k in pr if k not in writes]
        deps = []
        for k in reads:
            t = self.lastw.get(k)
            if t is not None:
                deps.append(t)
        for k in writes:
            t = self.lastw.get(k)
            if t is not None:
                deps.append(t)
            deps.extend(self.readers.get(k, {}).values())
        for t in deps:
            self._wait(eng, t)
        ins = fn(self.eng[eng])
        self.n_ops += 1
        if dma is not None:
            if dma not in self.dsem:
                self.dsem[dma] = self.stack.enter_context(self.nc.semaphore("d_" + str(len(self.dsem))))
                self.dcnt[dma] = 0
            self.dcnt[dma] += 16
            ins.then_inc(self.dsem[dma], 16)
            tok = ('D', dma, self.dcnt[dma])
        else:
            self.cnt[eng] += 1
            ins.then_inc(self.sem[eng], 1)
            tok = ('E', eng, self.cnt[eng])
        for k in writes:
            self.lastw[k] = tok
            self.readers[k] = {}
        for k in reads:
            self.readers.setdefault(k, {})[(tok[0], tok[1])] = tok
        if final:
            self.final.append(tok)
        return tok

    def barrier(self):
        toks = [('E', e, self.cnt[e]) for e in CENG if self.cnt[e] > 0]
        toks += [('D', k, v) for k, v in self.dcnt.items()]
        for e in self.eng:
            for t in toks:
                self._wait(e, t)

    def finish(self):
        for t in self.final:
            self._wait('sp', t)


def build(npool, stage=99):
    nc = bass.Bass("TRN2", target_bir_lowering=False)

    def din(name, shape, dt=F32):
        return nc.dram_tensor(name, shape, dt, kind="ExternalInput").ap()

    def dout(name, shape):
        return nc.dram_tensor(name, shape, F32, kind="ExternalOutput").ap()

    xw = din("xw", [2048, 2048]); xs = din("xs", [4, 2048])
    MINI = os.environ.get("K_MINI", "0") == "1"
    w_in0 = din("w_in0", [2048, 16 if MINI else 9224]); w_out0 = din("w_out0", [2048, 16 if MINI else 2048])
    w_in1 = din("w_in1", [2048, 16 if MINI else 6144]); w_out1 = din("w_out1", [2048, 16 if MINI else 2048])
    cst_d = din("cst", [128, NCST]); par_d = din("par", [128, NPAR])
    bsbc_d = din("bsbc", [128, 1024]); vgbc_d = din("vgbc", [128, 2048]); gfbc_d = din("gfbc", [128, 2048])
    wsT_d = din("wsT", [128, 8, 128]); aCT_d = din("aCT", [4, 256, 257])
    ck_d = din("ck", [npool * 128, 1024]); cv_d = din("cv", [npool * 128, 1024])
    ptb_d = din("ptb", [128, 128], I32)

    y_p = dout("y_p", [1024, 2048]); y_s = dout("y_s", [4, 2048])
    CTo_p = dout("CTo_p", [4, 256, 257]); mo_p = dout("mo_p", [1, 4])
    CTo_s = dout("CTo_s", [4, 256, 257]); mo_s = dout("mo_s", [1, 4])
    bk_p = dout("bk_p", [1024, 1024]); bv_p = dout("bv_p", [1024, 1024])
    bk_s = dout("bk_s", [4, 1024]); bv_s = dout("bv_s", [4, 1024]); cv_s = dout("cv_s", [4, 2048])

    top = ExitStack()
    S = Sched(nc, top)

    def sbt(stack, name, shape, dt=F32):
        return stack.enter_context(nc.sbuf_tensor("s_" + name, shape, dt))

    def pst(stack, name, shape, dt=F32):
        return stack.enter_context(nc.psum_tensor("p_" + name, shape, dt))

    def mm(out, lhsT, rhs, start, stop, r, w):
        S.op('pe', lambda e: e.matmul(out, lhsT, rhs, start=start, stop=stop), r, w)

    def tr(out, in_, ident, r, w):
        S.op('pe', lambda e: e.transpose(out, in_, ident), r, w)

    def act(out, in_, func, r, w, bias=None, scale=None, accum=None):
        kw = {}
        if bias is not None: kw['bias'] = bias
        if scale is not None: kw['scale'] = scale
        if accum is not None: kw['accum_out'] = accum
        S.op('act', lambda e: e.activation(out=out, in_=in_, func=func, **kw), r, w)

    def tt(eng, out, in0, in1, op, r, w):
        S.op(eng, lambda e: e.tensor_tensor(out=out, in0=in0, in1=in1, op=op), r, w)

    def ts(eng, out, in0, s1, s2, op0, op1, r, w):
        if s2 is None:
            S.op(eng, lambda e: e.tensor_scalar(out=out, in0=in0, scalar1=s1, scalar2=None, op0=op0), r, w)
        else:
            S.op(eng, lambda e: e.tensor_scalar(out=out, in0=in0, scalar1=s1, scalar2=s2, op0=op0, op1=op1), r, w)

    def stt(eng, out, in0, scalar, in1, op0, op1, r, w):
        S.op(eng, lambda e: e.scalar_tensor_tensor(out=out, in0=in0, scalar=scalar, in1=in1, op0=op0, op1=op1), r, w)

    def cp(eng, out, in_, r, w):
        if eng == 'act':
            act(out, in_, AF.Copy, r, w)
        else:
            S.op(eng, lambda e: e.tensor_copy(out=out, in_=in_), r, w)

    def rmax(out, in_, r, w):
        S.op('dve', lambda e: e.reduce_max(out=out, in_=in_, axis=AX), r, w)

    def dma(eng, out, in_, key, r, w, final=False):
        S.op(eng, lambda e: e.dma_start(out=out, in_=in_), r, w, dma=key, final=final)

    evac_rr = [0]

    def evac_eng():
        evac_rr[0] += 1
        ev = os.environ.get("K_EV", "")
        if ev:
            return ev
        return 'act' if evac_rr[0] % 2 else 'dve'

    def scaled_copy(eng, out, in_, scale, r, w):
        if eng == 'act':
            act(out, in_, AF.Copy, r, w, scale=scale)
        else:
            ts(eng, out, in_, scale, None, ALU.mult, None, r, w)

    cst = sbt(top, "cst", [128, C_DM], F32)
    cstb = sbt(top, "cstb", [128, 384 + 2048], BF16)
    par = sbt(top, "par", [128, NPAR], F32)
    bsbx = sbt(top, "bsbx", [128, 8], F32)
    mixT = sbt(top, "mixT", [128, 16, 1028], BF16)

    IDf = cst[:, C_ID:C_ID + 128]; TLE = cst[:, C_TLE:C_TLE + 128]; ONE = cst[:, C_ONE:C_ONE + 128]
    CM = cst[:, C_CM:C_CM + 128]; NTGf = cst[:, C_NTG:C_NTG + 128]; NONf = cst[:, C_NON:C_NON + 128]
    NM = cst[:, C_NM:C_NM + 32]
    IDb = cstb[:, 0:128]; NTGb = cstb[:, 128:256]; NONb = cstb[:, 256:384]

    def DMb(r):
        return cstb[:, 384 + r * 512: 384 + (r + 1) * 512]

    dma('sp', cst[:, :], cst_d[:, 0:C_DM], 'c0', [], ['cst'])
    dma('sp', par[:, :], par_d[:, :], 'c1', [], ['par'])
    dma('pool', cstb[:, 0:128], cst_d[:, C_ID:C_ID + 128], 'c2', [], ['cstb'])
    dma('pool', cstb[:, 128:384], cst_d[:, C_NTG:C_NTG + 256], 'c2', [], ['cstb'])
    dma('pool', cstb[:, 384:384 + 2048], cst_d[:, C_DM:C_DM + 2048], 'c2', [], ['cstb'])
    ts('dve', bsbx[:, :], par[:, P_BSB:P_BSB + 8], par[:, P_CTX:P_CTX + 1], None, ALU.add, None, ['par'], ['bsbx'])

    def tile_info(ti):
        return (ti * 128, 128) if ti < 16 else (2048, 4)

    def mixcol(ti):
        return (ti - 8) * 128 if ti < 16 else 1024

    NWS = 4
    wctr = [0]

    def load_w(ws, src_ap, ncols, shape3=None):
        si = wctr[0] % NWS
        wctr[0] += 1
        slot = ws[si]
        key = ('w', si)
        return slot, key, si

    L0 = ExitStack()
    hT = sbt(L0, "hT", [128, 16, 2052], BF16)
    wsl = [sbt(L0, "ws%d" % i, [128, 16, 256], BF16) for i in range(NWS)]

    W0LIST = []
    for h_ in range(4):
        W0LIST += [1024 + h_ * 256, 2048 + h_ * 256, 0 + h_ * 256, 3072 + h_ * 256, 4096 + h_ * 256]
    for hp_ in range(4):
        W0LIST += [5128 + hp_ * 256, 6152 + hp_ * 256, 7176 + hp_ * 256, 8200 + hp_ * 256]
    w0i = [0, 0]

    def w0_issue_upto(n):
        while w0i[0] < min(n, len(W0LIST)):
            i = w0i[0]
            si = i % NWS
            slot = wsl[si]
            src = w0cols(W0LIST[i], 256)
            S.op('pool', lambda e, slot=slot, src=src: e.dma_start(out=slot[:, :, :], in_=src), [], [('w', si)], dma=('w', si))
            w0i[0] += 1

    def wget(c0, prefetch=0):
        i = w0i[1]
        assert W0LIST[i] == c0, (i, W0LIST[i], c0)
        w0i[1] += 1
        w0_issue_upto(i + 1 + prefetch)
        return wsl[i % NWS], ('w', i % NWS)

    def w0cols(c0, ncols):
        return w_in0[:, c0:c0 + ncols].rearrange("(k p) j -> p k j", p=128)

    NT1 = int(os.environ.get('K_NT1', '17')) if stage >= 1 else 0
    with ExitStack() as P1:
        xt = [sbt(P1, "xt%d" % i, [128, 2048], F32) for i in range(2)]
        xn = [sbt(P1, "xn%d" % i, [128, 2048], F32) for i in range(2)]
        junk = sbt(P1, "junk", [128, 2048], BF16)
        st = sbt(P1, "st1", [128, 8], F32)
        ptr = [pst(P1, "ptr%d" % i, [128, 512], F32) for i in range(4)]
        for ti in range(NT1):
            tok0, L = tile_info(ti)
            sl = ti % 2
            src = xw[tok0:tok0 + 128, :] if ti < 16 else xs[:, :]
            PL = int(os.environ.get("K_P1", "9"))
            dma('sp', xt[sl][:L, :], src, ('xt', sl), [], [('xt', sl)])
            if PL >= 1:
                act(junk[:L, :], xt[sl][:L, :], AF.Square, [('xt', sl)], ['junk', 'ss'], accum=st[:L, 0:1])
            if PL >= 2:
                ts('dve', st[:L, 1:2], st[:L, 0:1], 1.0 / 2048, EPS, ALU.mult, ALU.add, ['ss'], ['ms'])
                act(st[:L, 2:3], st[:L, 1:2], AF.Ln, ['ms'], ['ln'])
                act(st[:L, 3:4], st[:L, 2:3], AF.Exp, ['ln'], ['rstd'], scale=-0.5)
            if PL >= 3:
                ts('dve', xn[sl][:L, :], xt[sl][:L, :], st[:L, 3:4], None, ALU.mult, None, [('xt', sl), 'rstd'], [('xn', sl)])
            for half in range(4):
                for j in range(4):
                    kc = half * 4 + j
                    if PL >= 4:
                        tr(ptr[half][:, j * 128:j * 128 + L], xn[sl][:L, kc * 128:(kc + 1) * 128], IDf[:L, :L],
                           [('xn', sl), 'cst'], [('ptr', half, j)])
                if PL >= 5 and os.environ.get("K_T") == "A":
                    act(junk[:, 0:512], ptr[half][:, :], AF.Copy, [('ptr', half, j) for j in range(4)], ['junk'])
                elif PL >= 5 and os.environ.get("K_T") == "B":
                    for j in range(4):
                        act(junk[:, j * 128:(j + 1) * 128], ptr[half][:, j * 128:(j + 1) * 128], AF.Copy, [('ptr', half, j)], ['junk'])
                elif PL >= 5 and os.environ.get("K_T") == "C":
                    act(junk[:, 0:512], ptr[half][:, :], AF.Copy, [('ptr', half, j) for j in range(4)] + ['par'], ['junk'], scale=par[:, 0:1])
                elif PL >= 5:
                    for j in range(4):
                        kc = half * 4 + j
                        scaled_copy(evac_eng(), hT[:, kc, tok0:tok0 + L], ptr[half][:, j * 128:j * 128 + L],
                                    par[:, P_G0 + kc:P_G0 + kc + 1], [('ptr', half, j), 'par'], [('hT', ti)])
    S.barrier()

    def hT_reads(t0, n):
        tiles = set()
        for t in range(t0, t0 + n, 4):
            tiles.add(min(t // 128, 16))
        tiles.add(min((t0 + n - 1) // 128, 16))
        return [('hT', t) for t in sorted(tiles)]

    def proj_fm(ps, pkey, slot, wkey, c0, t0, n):
        rd = hT_reads(t0, n) + [wkey]
        for kc in range(16):
            mm(ps[:, 0:n], slot[:, kc, c0:c0 + 128], hT[:, kc, t0:t0 + n], kc == 0, kc == 15, rd, [pkey])

    def proj_tm(ps, pkey, slot, wkey, c0, ncols, ti):
        tok0, L = tile_info(ti)
        rd = [('hT', ti), wkey]
        for kc in range(16):
            mm(ps[:L, 0:ncols], hT[:, kc, tok0:tok0 + L], slot[:, kc, c0:c0 + ncols], kc == 0, kc == 15, rd, [pkey])

    if stage >= 2:
        with ExitStack() as SA:
            wg = sbt(SA, "wg", [128, 16, 8], BF16)
            IG = sbt(SA, "IG", [128, 17, 4], F32); LF = sbt(SA, "LF", [128, 17, 4], F32)
            Aa = sbt(SA, "Aa", [128, 17, 4], F32); Bc = sbt(SA, "Bc", [128, 17, 4], F32)
            BT = sbt(SA, "BT", [128, 17, 4], F32)
            gt = sbt(SA, "gt", [128, 16], F32)
            qT = sbt(SA, "qT", [128, 2, 1028], BF16); kT = sbt(SA, "kT", [128, 2, 1028], BF16)
            kk = sbt(SA, "kk", [128, 17, 256], BF16); vv = sbt(SA, "vv", [128, 17, 257], BF16)
            og = sbt(SA, "og", [128, 9, 256], F32)
            sgt = sbt(SA, "sgt", [128, 512], F32)
            CT = sbt(SA, "CT", [128, 2, 257], F32); CTb = sbt(SA, "CTb", [128, 2, 257], BF16)
            m0 = sbt(SA, "m0", [128, 1], F32)
            dg = sbt(SA, "dg", [128, 128], F32); Am = sbt(SA, "Am", [128, 128], F32)
            Dm = sbt(SA, "Dm", [128, 128], F32); Sw = sbt(SA, "Sw", [128, 128], BF16)
            SwT = sbt(SA, "SwT", [128, 128], BF16)
            sc = sbt(SA, "sc", [128, 16], F32)
            tmpi = sbt(SA, "tmpi", [128, 256], F32); hh = sbt(SA, "hh", [128, 256], F32)
            mixa = sbt(SA, "mixa", [128, 256], BF16); vw = sbt(SA, "vw", [128, 257], BF16)
            pm = pst(SA, "pm", [128, 512], F32)
            pin = pst(SA, "pin", [128, 512], F32); pit = pst(SA, "pit", [128, 512], F32)
            pu = [pst(SA, "pu%d" % i, [128, 512], F32) for i in range(2)]
            pj = [pst(SA, "pj%d" % i, [128, 512], F32) for i in range(2)]
            ptb = pst(SA, "ptb", [128, 1024], BF16)

            S.op('pool', lambda e: e.dma_start(out=wg[:, :, :], in_=w0cols(5120, 8)), [], ['wg'], dma='wg')
            for ti in range(17):
                tok0, L = tile_info(ti)
                for kc in range(16):
                    mm(pm[:L, 256:264], hT[:, kc, tok0:tok0 + L], wg[:, kc, :], kc == 0, kc == 15, [('hT', ti), 'wg'], ['pm_g'])
                tt('dve', IG[:L, ti, :], pm[:L, 256:260], par[:L, P_BI:P_BI + 4], ALU.add, ['pm_g', 'par'], [('IG', ti)])
                tt('dve', gt[:L, 0:4], pm[:L, 260:264], par[:L, P_BF:P_BF + 4], ALU.add, ['pm_g', 'par'], ['gt0'])
                act(gt[:L, 4:8], gt[:L, 0:4], AF.Exp, ['gt0'], ['gt1'], scale=-1.0)
                act(gt[:L, 8:12], gt[:L, 4:8], AF.Ln, ['gt1'], ['gt2'], bias=1.0)
                ts('dve', LF[:L, ti, :], gt[:L, 8:12], -1.0, None, ALU.mult, None, ['gt2'], [('LF', ti)])
                mm(pm[:L, 268:272], TLE[:L, :L], LF[:L, ti, :], True, True, [('LF', ti), 'cst'], ['pm_b'])
                mm(pm[:, 264:268], ONE[:L, :], LF[:L, ti, :], True, True, [('LF', ti), 'cst'], ['pm_bt'])
                tt('dve', Aa[:L, ti, :], IG[:L, ti, :], pm[:L, 268:272], ALU.subtract, [('IG', ti), 'pm_b'], [('Aa', ti)])
                cp('dve', Bc[:L, ti, :], pm[:L, 268:272], ['pm_b'], [('Bc', ti)])
                cp('dve', BT[:, ti, :], pm[:, 264:268], ['pm_bt'], [('BT', ti)])

            pjr = [0]

            def nextpj():
                pjr[0] += 1
                i = pjr[0] % 2
                return pj[i], ('pj', i)

            from collections import deque
            Q = deque()
            credit = [0.0, 0.0]

            def pump():
                credit[0] += credit[1]
                while credit[0] >= 1.0 and Q:
                    credit[0] -= 1.0
                    Q.popleft()()

            def flush():
                while Q:
                    Q.popleft()()
                credit[0] = 0.0

            S.op('pool', lambda e: e.memset(vv[:, :, 256:257], 1.0), [], ['vv'])

            def items_for(h):
                W = {}
                base = {'k': 1024, 'v': 2048, 'q': 0, 'o': 3072, 'g': 4096}

                def need(name, prefetch=0):
                    if name not in W:
                        W[name] = wget(base[name] + h * 256, prefetch=prefetch)
                    return W[name]

                def kv(ti):
                    def f():
                        wk_, wkk = need('k')
                        wv, wvk = need('v', prefetch=2)
                        tok0, L = tile_info(ti)
                        ps, pk = nextpj()
                        proj_tm(ps, pk, wk_, wkk, 0, 256, ti)
                        scaled_copy(evac_eng(), kk[:L, ti, :], ps[:L, 0:256], 1.0 / 16, [pk], [('kk', ti)])
                        ps, pk = nextpj()
                        proj_tm(ps, pk, wv, wvk, 0, 256, ti)
                        cp(evac_eng(), vv[:L, ti, 0:256], ps[:L, 0:256], [pk, 'vv'], [('vv', ti)])
                    return f

                def fm(c2, t0, n, mc, which):
                    def f():
                        ps, pk = nextpj()
                        if which == 'q':
                            wq, wqk = need('q', prefetch=1)
                            proj_fm(ps, pk, wq, wqk, c2 * 128, t0, n)
                            cp(evac_eng(), qT[:, c2, mc:mc + n], ps[:, 0:n], [pk], ['qT'])
                        else:
                            wk_, wkk = need('k')
                            proj_fm(ps, pk, wk_, wkk, c2 * 128, t0, n)
                            scaled_copy(evac_eng(), kT[:, c2, mc:mc + n], ps[:, 0:n], 1.0 / 16, [pk], ['kT'])
                    return f

                def ogi(ti):
                    def f():
                        wo, wok = need('o')
                        wgg, wggk = need('g')
                        tok0, L = tile_info(ti)
                        ps, pk = nextpj()
                        proj_tm(ps, pk, wo, wok, 0, 256, ti)
                        ps2, pk2 = nextpj()
                        proj_tm(ps2, pk2, wgg, wggk, 0, 256, ti)
                        act(sgt[:L, 0:256], ps[:L, 0:256], AF.Exp, [pk], ['sgt0'], scale=-1.0)
                        act(sgt[:L, 256:512], ps2[:L, 0:256], AF.Exp, [pk2], ['sgt1'], scale=-1.0)
                        act(sgt[:L, :], sgt[:L, :], AF.Ln, ['sgt0', 'sgt1'], ['sgt0', 'sgt1'], bias=1.0)
                        tt('dve', sgt[:L, 0:256], sgt[:L, 0:256], sgt[:L, 256:512], ALU.add, ['sgt0', 'sgt1'], ['sgt0'])
                        act(sgt[:L, 0:256], sgt[:L, 0:256], AF.Exp, ['sgt0'], ['sgt0'], scale=-1.0)
                        tt('dve', og[:L, ti - 8, :], sgt[:L, 0:256], ps2[:L, 0:256], ALU.mult, ['sgt0', pk2], [('og', ti)])
                    return f

                kv_lo = [kv(t) for t in range(8)]
                q1 = []
                for c2 in range(2):
                    for (t0, n, mc) in ((1024, 512, 0), (1536, 512, 512), (2048, 4, 1024)):
                        q1.append(fm(c2, t0, n, mc, 'q'))
                        q1.append(fm(c2, t0, n, mc, 'k'))
                q1 += [kv(t) for t in range(8, 17)]
                q1 += [ogi(t) for t in range(8, 17)]
                return kv_lo, q1

            def step(h, ti):
                tok0, L = tile_info(ti)
                full = ti >= 8
                if ti == 16:
                    for kc in range(2):
                        dma('sp', CT[:, kc, :], aCT_d[h, kc * 128:(kc + 1) * 128, :], 'st_in', [], ['CT'])
                    cp('dve', m0[:, :], par[:, P_AM + h:P_AM + h + 1], ['par'], ['m0'])
                    cp('act', CTb[:, :, :], CT[:, :, :], ['CT'], ['CTb'])
                a_col = Aa[:L, ti, h:h + 1]
                ts('dve', dg[:L, :L], IDf[:L, :L], a_col, None, ALU.mult, None, [('Aa', ti), 'cst'], ['dg'])
                mm(pm[:, 0:L], ONE[:L, :], dg[:L, :L], True, True, ['dg', 'cst'], ['pm_a'])
                pump()
                rmax(sc[:, 0:1], pm[:, 0:L], ['pm_a'], ['sc0'])
                ts('dve', sc[:, 1:2], sc[:, 0:1], m0[:, 0:1], -1.0, ALU.max, ALU.mult, ['sc0', 'm0'], ['nml'])
                if full:
                    mc = mixcol(ti)
                    tt('dve', Am[:L, :L], pm[:L, 0:L], CM[:L, :L], ALU.add, ['pm_a', 'cst'], ['Am'])
                    rmax(sc[:L, 2:3], Am[:L, :L], ['Am'], ['sc2'])
                    ts('dve', sc[:L, 3:4], sc[:L, 2:3], m0[:L, 0:1], -1.0, ALU.max, ALU.mult, ['sc2', 'm0'], ['negM'])
                    act(Dm[:L, :L], Am[:L, :L], AF.Exp, ['Am', 'negM'], ['Dm'], bias=sc[:L, 3:4])
                    for kc in range(2):
                        mm(pm[:L, 128:128 + L], qT[:, kc, mc:mc + L], kT[:, kc, mc:mc + L], kc == 0, kc == 1, ['qT', 'kT'], ['pm_s'])
                    pump()
                    tt('dve', Sw[:L, :L], pm[:L, 128:128 + L], Dm[:L, :L], ALU.mult, ['pm_s', 'Dm'], ['Sw'])
                    tr(ptb[:L, 0:L], Sw[:L, :L], IDb[:L, :L], ['Sw', 'cstb'], ['ptb0'])
                    pump()
                    cp('act', SwT[:L, :L], ptb[:L, 0:L], ['ptb0'], ['SwT'])
                    mm(pin[:L, 0:257], SwT[:L, :L], vv[:L, ti, :], True, True, ['SwT', ('vv', ti)], ['pin'])
                    for kc in range(2):
                        mm(pit[:L, 0:257], qT[:, kc, mc:mc + L], CTb[:, kc, :], kc == 0, kc == 1, ['qT', 'CTb'], ['pit'])
                    act(sc[:L, 4:5], sc[:L, 3:4], AF.Exp, ['negM', 'm0'], ['wc'], bias=m0[:L, 0:1])
                    pump()
                    def back():
                        cp('dve', sc[:L, 5:6], pin[:L, 256:257], ['pin'], ['c1'])
                        stt('dve', sc[:L, 6:7], pit[:L, 256:257], sc[:L, 4:5], sc[:L, 5:6], ALU.mult, ALU.add, ['pit', 'wc', 'c1'], ['den'])
                        act(sc[:L, 7:8], sc[:L, 6:7], AF.Abs, ['den'], ['aden'])
                        act(sc[:L, 8:9], Bc[:L, ti, h:h + 1], AF.Exp, [('Bc', ti), 'negM'], ['lowb'], bias=sc[:L, 3:4], scale=-1.0)
                        tt('dve', sc[:L, 9:10], sc[:L, 7:8], sc[:L, 8:9], ALU.max, ['aden', 'lowb'], ['dmax'])
                        S.op('dve', lambda e, L=L: e.reciprocal(out=sc[:L, 10:11], in_=sc[:L, 9:10]), ['dmax'], ['rden'])
                        tt('dve', sc[:L, 11:12], sc[:L, 10:11], sc[:L, 4:5], ALU.mult, ['rden', 'wc'], ['wr'])
                        act(tmpi[:L, :], pit[:L, 0:256], AF.Copy, ['pit', 'wr'], ['tmpi'], scale=sc[:L, 11:12])
                        stt('dve', hh[:L, :], pin[:L, 0:256], sc[:L, 10:11], tmpi[:L, :], ALU.mult, ALU.add, ['pin', 'rden', 'tmpi'], ['hh'])
                        tt('dve', mixa[:L, :], hh[:L, :], og[:L, ti - 8, :], ALU.mult, ['hh', ('og', ti)], ['mixa'])
                        pump()
                        for j in range(2):
                            tr(ptb[:, 256 + j * 128:256 + j * 128 + L], mixa[:L, j * 128:(j + 1) * 128], IDb[:L, :L], ['mixa', 'cstb'], [('ptb1', j)])
                            cp(evac_eng(), mixT[:, h * 2 + j, mc:mc + L], ptb[:, 256 + j * 128:256 + j * 128 + L], [('ptb1', j)], ['mixT'])
                act(sc[:L, 12:13], a_col, AF.Exp, [('Aa', ti), 'nml'], ['wl'], bias=sc[:L, 1:2])
                act(sc[:, 13:14], sc[:, 1:2], AF.Exp, ['nml', 'm0'], ['wcl'], bias=m0[:, 0:1])
                ts('dve', vw[:L, :], vv[:L, ti, :], sc[:L, 12:13], None, ALU.mult, None, [('vv', ti), 'wl'], ['vw'])
                pump()
                for kc in range(2):
                    mm(pu[kc][:, 0:257], kk[:L, ti, kc * 128:(kc + 1) * 128], vw[:L, :], True, True, [('kk', ti), 'vw'], [('pu', kc)])
                    stt('dve', CT[:, kc, :], CT[:, kc, :], sc[:, 13:14], pu[kc][:, 0:257], ALU.mult, ALU.add, ['CT', 'wcl', ('pu', kc)], ['CT'])
                tt('dve', m0[:, :], BT[:, ti, h:h + 1], sc[:, 1:2], ALU.subtract, [('BT', ti), 'nml'], ['m0'])
                pump()
                if ti == 7:
                    ts('dve', CT[:, :, :], CT[:, :, :], par[:, P_FLAG:P_FLAG + 1], None, ALU.mult, None, ['CT', 'par'], ['CT'])
                    ts('dve', m0[:, :], m0[:, :], par[:, P_FLAG:P_FLAG + 1], None, ALU.mult, None, ['m0', 'par'], ['m0'])
                if ti < 15:
                    cp('act', CTb[:, :, :], CT[:, :, :], ['CT'], ['CTb'])
                if ti == 15 or ti == 16:
                    dst = CTo_p if ti == 15 else CTo_s
                    mdst = mo_p if ti == 15 else mo_s
                    for kc in range(2):
                        dma('sp', dst[h, kc * 128:(kc + 1) * 128, :], CT[:, kc, :], 'st_out', ['CT'], [], final=True)
                    dma('sp', mdst[0:1, h:h + 1], m0[0:1, 0:1], 'st_out', ['m0'], [], final=True)

                if full:
                    back()

            ITEMS = [items_for(h) for h in range(4)]
            for f in ITEMS[0][0]:
                f()
            for h in range(4):
                Q.extend(ITEMS[h][1])
                credit[0] = 0.0
                credit[1] = len(Q) / (8 * 3.0) + 0.01
                S.op('pool', lambda e: e.memset(CT[:, :, :], 0.0), [], ['CT'])
                S.op('pool', lambda e: e.memset(CTb[:, :, :], 0.0), [], ['CTb'])
                S.op('pool', lambda e: e.memset(m0[:, :], 0.0), [], ['m0'])
                for ti in range(8):
                    step(h, ti)
                flush()
                if h < 3:
                    Q.extend(ITEMS[h + 1][0])
                    credit[1] = len(Q) / (9 * 7.0) + 0.01
                for ti in range(8, 17):
                    step(h, ti)
                flush()
            w0_issue_upto(w0i[1] + 4)
        S.barrier()

    if stage >= 3:
        with ExitStack() as SB:
            qTb = sbt(SB, "qTb", [128, 2, 1028], BF16); kTb = sbt(SB, "kTb", [128, 2, 2052], BF16)
            vb = sbt(SB, "vb", [128, 17, 256], BF16); sgT = sbt(SB, "sgT", [128, 2, 1028], BF16)
            stg = [sbt(SB, "stg%d" % i, [128, 256], F32) for i in range(4)]
            gtmp = sbt(SB, "gtmp", [128, 512], F32)
            qTs = sbt(SB, "qTs", [128, 8, 4], BF16); kTs = sbt(SB, "kTs", [128, 8, 4], BF16)
            vs = sbt(SB, "vs", [4, 1024], BF16)
            gsT = sbt(SB, "gsT", [128, 8, 4], BF16)
            pj = [pst(SB, "pjb%d" % i, [128, 512], F32) for i in range(2)]
            ATT = ExitStack()
            ee = [sbt(ATT, "ee%d" % g, [128, 512], F32) for g in range(2)]
            Lb = [[sbt(ATT, "Lb%d_%d" % (g, i), [128, 512], BF16) for i in range(2)] for g in range(2)]
            aa = [[sbt(ATT, "aa%d_%d" % (g, i), [128, 512], BF16) for i in range(2)] for g in range(2)]
            Acc = [sbt(ATT, "Acc%d" % g, [128, 512], F32) for g in range(2)]
            Accb = [[sbt(ATT, "Accb%d_%d" % (g, i), [128, 512], BF16) for i in range(2)] for g in range(2)]
            pP = [[pst(ATT, "pP%d_%d" % (g, i), [128, 512], F32) for i in range(2)] for g in range(2)]
            pO = [pst(ATT, "pO%d" % g, [128, 512], F32) for g in range(2)]
            pjr = [0]

            def nextpj():
                pjr[0] += 1
                i = pjr[0] % 2
                return pj[i], ('pj', i)

            stgr = [0]

            def nextstg():
                stgr[0] += 1
                i = stgr[0] % 4
                return stg[i], ('stg', i)

            SCL = float(128 ** -0.5)
            ucount = [0]
            for hp in range(4):
                wq, wqk = wget(5128 + hp * 256)
                wk_, wkk = wget(6152 + hp * 256)
                wv, wvk = wget(7176 + hp * 256)
                wgb, wgbk = wget(8200 + hp * 256)
                for hh_ in range(2):
                    for (t0, n, mc) in ((1024, 512, 0), (1536, 512, 512), (2048, 4, 1024)):
                        ps, pk = nextpj()
                        proj_fm(ps, pk, wq, wqk, hh_ * 128, t0, n)
                        scaled_copy(evac_eng(), qTb[:, hh_, mc:mc + n], ps[:, 0:n], SCL, [pk], ['qTb'])
                        ps, pk = nextpj()
                        proj_fm(ps, pk, wgb, wgbk, hh_ * 128, t0, n)
                        act(gtmp[:, 0:n], ps[:, 0:n], AF.Exp, [pk], ['gtmp'], scale=-1.0)
                        act(gtmp[:, 0:n], gtmp[:, 0:n], AF.Ln, ['gtmp'], ['gtmp'], bias=1.0)
                        act(gtmp[:, 0:n], gtmp[:, 0:n], AF.Exp, ['gtmp'], ['gtmp'], scale=-1.0)
                        tt('dve', sgT[:, hh_, mc:mc + n], gtmp[:, 0:n], ps[:, 0:n], ALU.mult, ['gtmp', pk], ['sgT'])
                    for (t0, n) in ((0, 512), (512, 512), (1024, 512), (1536, 512), (2048, 4)):
                        ps, pk = nextpj()
                        proj_fm(ps, pk, wk_, wkk, hh_ * 128, t0, n)
                        cp(evac_eng(), kTb[:, hh_, t0:t0 + n], ps[:, 0:n], [pk], ['kTb'])
                    cp('dve', qTs[:, hp * 2 + hh_, :], qTb[:, hh_, 1024:1028], ['qTb'], ['qTs'])
                    cp('dve', kTs[:, hp * 2 + hh_, :], kTb[:, hh_, 2048:2052], ['kTb'], ['kTs'])
                    cp('dve', gsT[:, hp * 2 + hh_, :], sgT[:, hh_, 1024:1028], ['sgT'], ['gsT'])
                for ti in range(17):
                    tok0, L = tile_info(ti)
                    ps, pk = nextpj()
                    proj_tm(ps, pk, wv, wvk, 0, 256, ti)
                    cp('act', vb[:L, ti, :], ps[:L, 0:256], [pk], [('vb', ti)])
                    if ti >= 8:
                        sg_, sgk = nextstg()
                        cp('dve', sg_[:L, :], ps[:L, 0:256], [pk], [sgk])
                        if ti < 16:
                            dma('sp', bv_p[(ti - 8) * 128:(ti - 7) * 128, hp * 256:(hp + 1) * 256], sg_[:, :], sgk, [sgk], [], final=True)
                        else:
                            dma('sp', bv_s[:, hp * 256:(hp + 1) * 256], sg_[:4, :], sgk, [sgk], [], final=True)
                            cp('dve', vs[:4, hp * 256:(hp + 1) * 256], ps[:4, 0:256], [pk], ['vs'])
                        ps, pk = nextpj()
                        proj_tm(ps, pk, wk_, wkk, 0, 256, ti)
                        sg_, sgk = nextstg()
                        cp(evac_eng(), sg_[:L, :], ps[:L, 0:256], [pk], [sgk])
                        if ti < 16:
                            dma('sp', bk_p[(ti - 8) * 128:(ti - 7) * 128, hp * 256:(hp + 1) * 256], sg_[:, :], sgk, [sgk], [], final=True)
                        else:
                            dma('sp', bk_s[:, hp * 256:(hp + 1) * 256], sg_[:4, :], sgk, [sgk], [], final=True)
                w0_issue_upto(w0i[1] + 4)
                if stage >= 4:
                    for G in range(2):
                        blocks = [(8 + jb, jb - 4 * G) for jb in range(4 * G + 3, -1, -1)] + [(kt, None) for kt in range(7, -1, -1)]
                        nb = len(blocks)
                        U = [(s_, bi) for bi in range(nb) for s_ in range(2)]

                        def info(k):
                            s_, bi = U[k]
                            kt, r = blocks[bi]
                            own = kt >= 8
                            h = hp * 2 + s_
                            bias = par[:, P_BSB + h:P_BSB + h + 1] if own else bsbx[:, h:h + 1]
                            bkey = 'par' if own else 'bsbx'
                            return s_, bi, kt, r, own, bias, bkey, bi % 2

                        def a1(k):
                            s_, bi, kt, r, own, bias, bkey, u = info(k)
                            pk = ('pP', s_, u)
                            mm(pP[s_][u][:, :], kTb[:, s_, kt * 128:(kt + 1) * 128], qTb[:, s_, G * 512:(G + 1) * 512], True, False, ['kTb', 'qTb'], [pk])
                            if own and r >= 0:
                                mm(pP[s_][u][:, :], IDb, DMb(r), False, False, ['cstb'], [pk])

                        def a2(k):
                            s_, bi, kt, r, own, bias, bkey, u = info(k)
                            pk = ('pP', s_, u)
                            act(ee[s_][:, :], pP[s_][u][:, :], AF.Exp, [pk, bkey], [('ee', s_)], bias=bias)
                            act(Lb[s_][u][:, :], ee[s_][:, :], AF.Ln, [('ee', s_)], [('Lb', s_, u)], bias=1.0)
                            if bi < nb - 1:
                                if bi == 0:
                                    cp('dve', Accb[s_][1 - u][:, :], Lb[s_][u][:, :], [('Lb', s_, u)], [('Accb', s_, 1 - u)])
                                    cp('dve', Acc[s_][:, :], Lb[s_][u][:, :], [('Lb', s_, u)], [('Acc', s_)])
                                else:
                                    tt('dve', Accb[s_][1 - u][:, :], Acc[s_][:, :], Lb[s_][u][:, :], ALU.add, [('Acc', s_), ('Lb', s_, u)], [('Accb', s_, 1 - u)])
                                    tt('dve', Acc[s_][:, :], Acc[s_][:, :], Lb[s_][u][:, :], ALU.add, [('Acc', s_), ('Lb', s_, u)], [('Acc', s_)])

                        def a3(k):
                            s_, bi, kt, r, own, bias, bkey, u = info(k)
                            pk = ('pP', s_, u)
                            mm(pP[s_][u][:, :], NTGb, Lb[s_][u][:, :], False, bi == 0, [('Lb', s_, u), 'cstb'], [pk])
                            if bi > 0:
                                mm(pP[s_][u][:, :], NONb, Accb[s_][u][:, :], False, True, [('Accb', s_, u), 'cstb'], [pk])

                        def a4(k):
                            s_, bi, kt, r, own, bias, bkey, u = info(k)
                            act(aa[s_][u][:, :], pP[s_][u][:, :], AF.Exp, [('pP', s_, u), bkey], [('aa', s_, u)], bias=bias)

                        def a5(k):
                            s_, bi, kt, r, own, bias, bkey, u = info(k)
                            mm(pO[s_][:, :], vb[:, kt, s_ * 128:(s_ + 1) * 128], aa[s_][u][:, :], bi == 0, bi == nb - 1, [('aa', s_, u), ('vb', kt)], [('pO', s_)])

                        NU = len(U)
                        a1(0); a1(1); a2(0)
                        for k in range(NU):
                            if k + 2 < NU: a1(k + 2)
                            if k + 1 < NU: a2(k + 1)
                            a3(k)
                            a4(k)
                            if k >= 1: a5(k - 1)
                        a5(NU - 1)
                        for s_ in range(2):
                            h = hp * 2 + s_
                            tt('dve', mixT[:, 8 + h, G * 512:(G + 1) * 512], pO[s_][:, :], sgT[:, s_, G * 512:(G + 1) * 512], ALU.mult, [('pO', s_), 'sgT'], ['mixT'])
            S.barrier()
            ATT.close()
            S.barrier()
            if stage >= 5:
                with ExitStack() as SS:
                    NKS = 6
                    kpg = [sbt(SS, "kpg%d" % i, [128, 1024], BF16) for i in range(NKS)]
                    vpg = [sbt(SS, "vpg%d" % i, [128, 1024], BF16) for i in range(NKS)]
                    KTt = [sbt(SS, "KTt%d" % i, [128, 1024], BF16) for i in range(2)]
                    ptbi = sbt(SS, "ptbi", [128, 128], I32); idx = sbt(SS, "idx", [128, 128], I32)
                    zb = [sbt(SS, "zb%d" % i, [128, 32], F32) for i in range(2)]
                    es = [sbt(SS, "es%d" % i, [128, 32], F32) for i in range(2)]
                    Ls = [sbt(SS, "Ls%d" % i, [128, 32], F32) for i in range(2)]
                    rr = [sbt(SS, "rr%d" % i, [128, 32], F32) for i in range(2)]
                    asb = [sbt(SS, "asb%d" % i, [128, 32], BF16) for i in range(2)]
                    AccS = sbt(SS, "AccS", [128, 32], F32)
                    Os = sbt(SS, "Os", [4, 1024], F32)
                    sgs = sbt(SS, "sgs", [128, 32], F32)
                    pkt = [pst(SS, "pkt%d" % i, [128, 1024], BF16) for i in range(2)]
                    pzb = [pst(SS, "pzb%d" % i, [128, 512], F32) for i in range(2)]
                    posb = [pst(SS, "posb%d" % i, [128, 512], F32) for i in range(2)]
                    dma('sp', ptbi[:, :], ptb_d[:, :], 'ptb', [], ['ptbi'])
                    ts('dve', idx[:, :], ptbi[:, :], 128.0, par[:, P_PIDX:P_PIDX + 1], ALU.mult, ALU.add, ['ptbi', 'par'], ['idx'])
                    bsbs = par[:, P_BSBS:P_BSBS + 32]
                    S.op('pool', lambda e: e.memset(AccS[:, :], 0.0), [], ['AccS'])

                    NPG = 128
                    PF = NKS - 2

                    def issue(i):
                        j = NPG - 1 - i
                        sl = i % NKS
                        S.op('pool', lambda e: e.indirect_dma_start(out=kpg[sl][:, :], out_offset=None, in_=ck_d[:, :],
                                                                    in_offset=bass.IndirectOffsetOnAxis(ap=idx[:, j:j + 1], axis=0)),
                             ['idx'], [('kpg', sl)], dma=('kpg', sl))
                        S.op('pool', lambda e: e.indirect_dma_start(out=vpg[sl][:, :], out_offset=None, in_=cv_d[:, :],
                                                                    in_offset=bass.IndirectOffsetOnAxis(ap=idx[:, j:j + 1], axis=0)),
                             ['idx'], [('vpg', sl)], dma=('vpg', sl))

                    for i in range(PF):
                        issue(i)

                    def st_elem1(u, Lk, newblk):
                        pz = pzb[u]; pzk = ('pzb', u)
                        tt('dve', zb[u][:Lk, :], pz[:Lk, 0:32], bsbs[:Lk, :], ALU.add, [pzk, 'par'], [('zb', u)])
                        if newblk:
                            tt('dve', zb[u][:Lk, :], zb[u][:Lk, :], NM[:Lk, :], ALU.add, [('zb', u), 'cst'], [('zb', u)])
                        act(es[u][:Lk, :], zb[u][:Lk, :], AF.Exp, [('zb', u)], [('es', u)])
                        act(Ls[u][:Lk, :], es[u][:Lk, :], AF.Ln, [('es', u)], [('Ls', u)], bias=1.0)

                    def st_cum(u, Lk, first, last, newblk):
                        pz = pzb[u]; pzk = ('pzb', u)
                        mm(pz[:Lk, 32:64], NTGf[:Lk, :Lk], Ls[u][:Lk, :], True, first, [('Ls', u), 'cst'], [pzk])
                        if not first:
                            mm(pz[:Lk, 32:64], NONf[:, :Lk], AccS[:, :], False, True, ['AccS', 'cst'], [pzk])
                        tt('dve', rr[u][:Lk, :], pz[:Lk, 32:64], zb[u][:Lk, :], ALU.add, [pzk, ('zb', u)], [('rr', u)])
                        act(asb[u][:Lk, :], rr[u][:Lk, :], AF.Exp, [('rr', u)], [('asb', u)])
                        if newblk:
                            cp('dve', AccS[:Lk, :], Ls[u][:Lk, :], [('Ls', u)], ['AccS'])
                        elif not last:
                            tt('dve', AccS[:, :], AccS[:, :], Ls[u][:, :], ALU.add, ['AccS', ('Ls', u)], ['AccS'])

                    def st_av(u, Lk, vsrc, vkeys, first, last):
                        for h in range(8):
                            mm(posb[h // 4][:4, (h % 4) * 128:(h % 4 + 1) * 128], asb[u][:Lk, h * 4:(h + 1) * 4], vsrc[:Lk, h * 128:(h + 1) * 128],
                               first, last, [('asb', u)] + vkeys, [('posb', h // 4)])

                    for h in range(8):
                        mm(pzb[1][:4, h * 4:(h + 1) * 4], kTs[:, h, :], qTs[:, h, :], True, True, ['kTs', 'qTs'], [('pzb', 1)])
                    st_elem1(1, 4, True)
                    st_cum(1, 4, True, False, True)
                    st_av(1, 4, vs, ['vs'], True, False)

                    def st_tr(i):
                        sl = i % NKS; u = i % 2
                        for h in range(8):
                            tr(pkt[u][:, h * 128:(h + 1) * 128], kpg[sl][:, h * 128:(h + 1) * 128], IDb, [('kpg', sl), 'cstb'], [('pkt', u)])
                        cp(evac_eng(), KTt[u][:, :], pkt[u][:, :], [('pkt', u)], [('KTt', u)])

                    def st_z(i):
                        u = i % 2
                        for h in range(8):
                            mm(pzb[u][:, h * 4:(h + 1) * 4], KTt[u][:, h * 128:(h + 1) * 128], qTs[:, h, :], True, True, [('KTt', u), 'qTs'], [('pzb', u)])
                        st_elem1(u, 128, False)

                    st_tr(0); st_tr(1); st_z(0)
                    for i in range(NPG):
                        if i + 2 < NPG: st_tr(i + 2)
                        if i + 1 < NPG: st_z(i + 1)
                        st_cum(i % 2, 128, False, i == NPG - 1, False)
                        if i >= 1:
                            st_av((i - 1) % 2, 128, vpg[(i - 1) % NKS], [('vpg', (i - 1) % NKS)], False, False)
                        if i + PF < NPG: issue(i + PF)
                    st_av((NPG - 1) % 2, 128, vpg[(NPG - 1) % NKS], [('vpg', (NPG - 1) % NKS)], False, True)
                    for q in range(2):
                        cp('dve', Os[:4, q * 512:(q + 1) * 512], posb[q][:4, :], [('posb', q)], ['Os'])
                    for h in range(8):
                        tr(pzb[0][:, 64 + h * 4:64 + (h + 1) * 4], Os[:4, h * 128:(h + 1) * 128], IDf[:4, :4], ['Os', 'cst'], [('pzb', 0)])
                    cp('dve', sgs[:, :], pzb[0][:, 64:96], [('pzb', 0)], ['sgs'])
                    tt('dve', mixT[:, 8:16, 1024:1028], sgs[:, :].rearrange("p (h t) -> p h t", t=4), gsT[:, :, :], ALU.mult, ['sgs', 'gsT'], ['mixT'])
        S.barrier()
    L0.close()
    S.barrier()

    if stage >= 6:
        L1s = ExitStack()
        x1 = sbt(L1s, "x1", [128, 9, 2048], F32)
        NW2 = 3
        ws2 = [sbt(L1s, "w2s%d" % i, [128, 4096], BF16) for i in range(NW2)]
        w2c = [0]

        W2LIST = []
        for cb_ in range(8):
            W2LIST.append((w_out0[:, cb_ * 256:(cb_ + 1) * 256].rearrange("(k p) j -> p k j", p=128), 256))
        for H_ in range(2):
            for vb__ in range(8):
                W2LIST.append((w_in1[:, 2048 + vb__ * 256:2048 + (vb__ + 1) * 256].rearrange("(k p) j -> p k j", p=128), 256))
            for g_ in range(8):
                W2LIST.append((w_in1[:, g_ * 256:(g_ + 1) * 256].rearrange("(k p) j -> p k j", p=128), 256))
                W2LIST.append((w_in1[:, 4096 + g_ * 256:4096 + (g_ + 1) * 256].rearrange("(k p) j -> p k j", p=128), 256))
                W2LIST.append((w_out1[g_ * 256:(g_ + 1) * 256, :].rearrange("(k p) j -> p k j", p=128), 2048))
        w2i = [0, 0]

        def w2_issue_upto(n):
            while w2i[0] < min(n, len(W2LIST)):
                i = w2i[0]
                si = i % NW2
                src, j = W2LIST[i]
                view = ws2[si][:, :].rearrange("p (k j) -> p k j", j=j)
                S.op('pool', lambda e, view=view, src=src: e.dma_start(out=view, in_=src), [], [('w2', si)], dma=('w2', si))
                w2i[0] += 1

        def wload2(src_unused, j, prefetch=0):
            i = w2i[1]
            assert W2LIST[i][1] == j
            w2i[1] += 1
            w2_issue_upto(i + 1 + prefetch)
            return ws2[i % NW2][:, :].rearrange("p (k j) -> p k j", j=j), ('w2', i % NW2)

        def lt_info(lt):
            return (lt * 128, 128) if lt < 8 else (1024, 4)

        with ExitStack() as P4:
            xsl = [sbt(P4, "xsl%d" % i, [128, 9, 256], F32) for i in range(2)]
            pj4 = [pst(P4, "pj4%d" % i, [128, 512], F32) for i in range(2)]
            cnt4 = 0
            for cb in range(8):
                slot, wk4 = wload2(None, 256, prefetch=2)
                xi = cb % 2
                dma('sp', xsl[xi][:, 0:8, :], xw[1024:2048, cb * 256:(cb + 1) * 256].rearrange("(t p) j -> p t j", p=128), ('xsl', xi), [], [('xsl', xi)])
                dma('sp', xsl[xi][:4, 8, :], xs[:, cb * 256:(cb + 1) * 256], ('xsl', xi), [], [('xsl', xi)])
                for lt in range(9):
                    c0, L = lt_info(lt)
                    cnt4 += 1
                    ps = pj4[cnt4 % 2]; pk = ('pj4', cnt4 % 2)
                    for kc in range(16):
                        mm(ps[:L, 0:256], mixT[:, kc, c0:c0 + L], slot[:, kc, :], kc == 0, kc == 15, ['mixT', wk4], [pk])
                    tt('dve', x1[:L, lt, cb * 256:(cb + 1) * 256], ps[:L, 0:256], xsl[xi][:L, lt, :], ALU.add, [pk, ('xsl', xi)], [('x1', lt)])
        S.barrier()

        with ExitStack() as LL:
            gv = sbt(LL, "gv", [128, 5, 2048], BF16)
            wmTs = sbt(LL, "wmTs", [128, 5, 1024], BF16)
            wmT = sbt(LL, "wmT", [128, 1024], BF16)
            big8 = sbt(LL, "big8", [128, 2048], F32)
            gbc = sbt(LL, "gbc", [128, 2048], F32)
            Bs = sbt(LL, "Bs", [128, 1024], F32)
            gu = sbt(LL, "gu", [128, 516], F32); th = sbt(LL, "th", [128, 516], F32); ug = sbt(LL, "ug", [128, 516], F32)
            svt = sbt(LL, "svt", [128, 128], F32)
            y1T = sbt(LL, "y1T", [128, 2, 516], BF16)
            xnb = sbt(LL, "xnb", [128, 2048], BF16)
            st2 = sbt(LL, "st2", [128, 16], F32)
            rv = sbt(LL, "rv", [128, 8], F32)
            ptr2 = [pst(LL, "ptr2%d" % i, [128, 1024], BF16) for i in range(2)]
            pa = [pst(LL, "pa%d" % i, [128, 512], F32) for i in range(2)]
            pb = [pst(LL, "pb%d" % i, [128, 512], F32) for i in range(2)]
            pc = [pst(LL, "pc%d" % i, [128, 512], F32) for i in range(2)]
            h1T = mixT

            dma('sp', Bs[:, :], bsbc_d[:, :], 'bs', [], ['Bs'])
            dma('sp', gbc[:, :], vgbc_d[:, :], 'gbc', [], ['gbc'])
            dma('sp', big8[:, 0:1024], wsT_d.rearrange("p g t -> p (g t)"), 'big8', [], ['big8'])
            for g in range(8):
                tt('dve', wmT[:, g * 128:(g + 1) * 128], big8[:, g * 128:(g + 1) * 128], TLE, ALU.mult, ['big8', 'cst'], ['wmT'])

            def rstd_of(src_ap, L, col_out, rkeys, wkey):
                act(big8[:L, :], src_ap, AF.Square, rkeys + ['big8'], ['big8', 'st2a'], accum=st2[:L, 0:1])
                ts('dve', st2[:L, 1:2], st2[:L, 0:1], 1.0 / 2048, EPS, ALU.mult, ALU.add, ['st2a'], ['st2b'])
                act(st2[:L, 2:3], st2[:L, 1:2], AF.Ln, ['st2b'], ['st2c'])
                act(col_out, st2[:L, 2:3], AF.Exp, ['st2c'], [wkey], scale=-0.5)

            cA = [0]; cB = [0]; cC = [0]
            for H in range(2):
                lts = list(range(4)) if H == 0 else list(range(4, 9))

                def lcol(lt):
                    return (lt - 4 * H) * 128

                for lt in lts:
                    c0, L = lt_info(lt)
                    rstd_of(x1[:L, lt, :], L, st2[:L, 3:4], [('x1', lt)], 'st2d')
                    ts('dve', xnb[:L, :], x1[:L, lt, :], st2[:L, 3:4], None, ALU.mult, None, [('x1', lt), 'st2d'], ['xnb'])
                    for half in range(2):
                        for j in range(8):
                            kc = half * 8 + j
                            tr(ptr2[half][:, j * 128:j * 128 + L], xnb[:L, kc * 128:(kc + 1) * 128], IDb[:L, :L], ['xnb', 'cstb'], [('ptr2', half, j)])
                        for j in range(8):
                            kc = half * 8 + j
                            scaled_copy(evac_eng(), h1T[:, kc, lcol(lt):lcol(lt) + L], ptr2[half][:, j * 128:j * 128 + L],
                                        par[:, P_G1 + kc:P_G1 + kc + 1], [('ptr2', half, j), 'par'], ['mixT'])
                for vb_ in range(8):
                    slot, wk1 = wload2(None, 256, prefetch=2)
                    for lt in lts:
                        c0, L = lt_info(lt)
                        cA[0] += 1
                        ps = pa[cA[0] % 2]; pk = ('pa', cA[0] % 2)
                        for kc in range(16):
                            mm(ps[:L, 0:256], h1T[:, kc, lcol(lt):lcol(lt) + L], slot[:, kc, :], kc == 0, kc == 15, ['mixT', wk1], [pk])
                        act(gv[:L, lt - 4 * H, vb_ * 256:(vb_ + 1) * 256], ps[:L, 0:256], AF.Gelu, [pk], [('gv', lt - 4 * H)])
                for lt in lts:
                    c0, L = lt_info(lt)
                    l = lt - 4 * H
                    rstd_of(gv[:L, l, :], L, rv[:L, l:l + 1], [('gv', l)], ('rv', l))
                    ts('dve', wmTs[:L, l, :], wmT[:L, :], rv[:L, l:l + 1], None, ALU.mult, None, ['wmT', ('rv', l)], [('wmTs', l)])
                if H == 1:
                    stt('dve', big8[:4, :], gv[:4, 4, :], rv[:4, 4:5], gbc[:4, :], ALU.mult, ALU.mult, [('gv', 4), ('rv', 4), 'gbc', 'big8'], ['big8'])
                    dma('sp', cv_s[:, :], big8[:4, :], 'big8', ['big8'], [], final=True)
                ngrp = [(0, 512)] if H == 0 else [(0, 512), (512, 4)]
                for g in range(8):
                    wu, wuk = wload2(None, 256, prefetch=0)
                    wg2, wg2k = wload2(None, 256, prefetch=1)
                    for c2 in range(2):
                        chunk = g * 2 + c2
                        for (lc0, n) in ngrp:
                            cB[0] += 1
                            ps = pb[cB[0] % 2]; pk = ('pb', cB[0] % 2)
                            for kc in range(16):
                                mm(ps[:, 0:n], wu[:, kc, c2 * 128:(c2 + 1) * 128], h1T[:, kc, lc0:lc0 + n], kc == 0, kc == 15, ['mixT', wuk], [pk])
                            act(gu[:, lc0:lc0 + n], ps[:, 0:n], AF.Gelu, [pk], ['gu'])
                            cB[0] += 1
                            ps = pb[cB[0] % 2]; pk = ('pb', cB[0] % 2)
                            for kc in range(16):
                                mm(ps[:, 0:n], wg2[:, kc, c2 * 128:(c2 + 1) * 128], h1T[:, kc, lc0:lc0 + n], kc == 0, kc == 15, ['mixT', wg2k], [pk])
                            act(th[:, lc0:lc0 + n], ps[:, 0:n], AF.Tanh, [pk], ['th'], scale=0.5)
                            ts('dve', th[:, lc0:lc0 + n], th[:, lc0:lc0 + n], 0.5, 0.5, ALU.mult, ALU.add, ['th'], ['th'])
                            tt('dve', th[:, lc0:lc0 + n], th[:, lc0:lc0 + n], ps[:, 0:n], ALU.mult, ['th', pk], ['th'])
                            tt('dve', ug[:, lc0:lc0 + n], gu[:, lc0:lc0 + n], th[:, lc0:lc0 + n], ALU.mult, ['gu', 'th'], ['ug'])
                        for lt in lts:
                            c0, L = lt_info(lt)
                            l = lt - 4 * H
                            cC[0] += 1
                            ps = pc[cC[0] % 2]; pk = ('pc', cC[0] % 2)
                            mm(ps[:, 0:L], gv[:L, l, chunk * 128:(chunk + 1) * 128], wmTs[:L, l, g * 128:g * 128 + L], True, True, [('gv', l), ('wmTs', l)], [pk])
                            stt('dve', svt[:, 0:L], ps[:, 0:L], par[:, P_VG + chunk:P_VG + chunk + 1], Bs[:, g * 128:g * 128 + L], ALU.mult, ALU.add, [pk, 'par', 'Bs'], ['svt'])
                            tt('dve', y1T[:, c2, lcol(lt):lcol(lt) + L], svt[:, 0:L], ug[:, lcol(lt):lcol(lt) + L], ALU.mult, ['svt', 'ug'], ['y1T'])
                    wo1, wo1k = wload2(None, 2048, prefetch=2)
                    for lt in lts:
                        c0, L = lt_info(lt)
                        for cb in range(4):
                            cC[0] += 1
                            ps = pc[cC[0] % 2]; pk = ('pc', cC[0] % 2)
                            for c2 in range(2):
                                mm(ps[:L, :], y1T[:, c2, lcol(lt):lcol(lt) + L], wo1[:, c2, cb * 512:(cb + 1) * 512], c2 == 0, c2 == 1, ['y1T', wo1k], [pk])
                            tt('dve', x1[:L, lt, cb * 512:(cb + 1) * 512], x1[:L, lt, cb * 512:(cb + 1) * 512], ps[:L, :], ALU.add, [('x1', lt), pk], [('x1', lt)])
            dma('sp', gbc[:, :], gfbc_d[:, :], 'gbc', [], ['gbc'])
            for lt in range(9):
                c0, L = lt_info(lt)
                rstd_of(x1[:L, lt, :], L, st2[:L, 4:5], [('x1', lt)], 'st2e')
                stt('dve', big8[:L, :], x1[:L, lt, :], st2[:L, 4:5], gbc[:L, :], ALU.mult, ALU.mult, [('x1', lt), 'st2e', 'gbc', 'big8'], ['big8'])
                if lt < 8:
                    dma('sp', y_p[lt * 128:(lt + 1) * 128, :], big8[:, :], 'big8', ['big8'], [], final=True)
                else:
                    dma('sp', y_s[:, :], big8[:4, :], 'big8', ['big8'], [], final=True)
        S.barrier()
        L1s.close()

    S.finish()
    top.close()
    return nc


def _consts():
    c = np.zeros((128, NCST), np.float32)
    i = np.arange(128)
    c[:, C_ID:C_ID + 128] = np.eye(128)
    c[:, C_TLE:C_TLE + 128] = (i[:, None] <= i[None, :])
    c[:, C_ONE:C_ONE + 128] = 1.0
    c[:, C_CM:C_CM + 128] = np.where(i[None, :] <= i[:, None], 0.0, NEG)
    c[:, C_NTG:C_NTG + 128] = -(i[:, None] >= i[None, :]).astype(np.float32)
    c[:, C_NON:C_NON + 128] = -1.0
    nm = np.zeros((128, 32), np.float32)
    for s in range(4):
        for h in range(8):
            for t in range(4):
                nm[s, h * 4 + t] = 0.0 if s < t else NEG
    c[:, C_NM:C_NM + 32] = nm
    tq = np.arange(512)
    for r in range(4):
        c[:, C_DM + r * 512:C_DM + (r + 1) * 512] = np.where((r * 128 + i[:, None]) < tq[None, :], 0.0, NEG)
    return c


_CACHE = {}


def kernel(**inp):
    stage = int(os.environ.get("K_STAGE", "99"))
    f32 = np.float32
    xp = np.asarray(inp["x_prompt"], f32); xsm = np.asarray(inp["x_sample"], f32)
    ck = np.asarray(inp["cache_b_k"], f32)[0]; cv = np.asarray(inp["cache_b_v"], f32)[0]
    npool = ck.shape[0]
    ck2 = np.ascontiguousarray(ck.reshape(npool * 128, 1024)); cv2 = np.ascontiguousarray(cv.reshape(npool * 128, 1024))
    pt = np.asarray(inp["page_table"], np.int32)
    w_in0 = np.ascontiguousarray(inp["even_w_in"][0], f32); w_out0 = np.ascontiguousarray(inp["even_w_out"][0], f32)
    w_in1 = np.ascontiguousarray(inp["odd_w_in"][0], f32); w_out1 = np.ascontiguousarray(inp["odd_w_out"][0], f32)
    if os.environ.get("K_MINI", "0") == "1":
        w_in0 = w_in0[:, :16].copy(); w_out0 = w_out0[:, :16].copy(); w_in1 = w_in1[:, :16].copy(); w_out1 = w_out1[:, :16].copy()
    cst = _consts()
    key = (npool, stage)
    if key not in _CACHE:
        _CACHE[key] = build(npool, stage)
    nc = _CACHE[key]
    aC = np.asarray(inp["state_a_C"], f32)[0]; an = np.asarray(inp["state_a_n"], f32)[0]; am = np.asarray(inp["state_a_m"], f32)[0]
    in_maps = []
    NCR = int(os.environ.get("K_NCORES", "8"))
    for c in range(NCR):
        b, hf = c // 2, c % 2
        if hf == 1:
            xw = xp[b]
        else:
            xw = np.concatenate([np.zeros((1024, 2048), f32), xp[b, :1024]], axis=0)
        par = np.zeros((128, NPAR), f32)
        par[:, P_G0:P_G0 + 16] = inp["even_norm"][0].reshape(16, 128).T
        par[:, P_G1:P_G1 + 16] = inp["odd_norm"][0].reshape(16, 128).T
        par[:, P_VG:P_VG + 16] = inp["odd_v_gain"][0].reshape(16, 128).T
        par[:, P_BI:P_BI + 4] = inp["even_b_i"][0][None, :]
        par[:, P_BF:P_BF + 4] = inp["even_b_f"][0][None, :]
        par[:, P_BSB:P_BSB + 8] = inp["even_b_sb"][0][None, :]
        par[:, P_BSBS:P_BSBS + 32] = np.repeat(inp["even_b_sb"][0], 4)[None, :]
        par[:, P_FLAG] = float(hf)
        par[:, P_CTX] = 0.0 if hf == 1 else NEG
        par[:, P_AM:P_AM + 4] = am[c][None, :]
        par[:, P_PIDX] = np.arange(128)
        aCT = np.concatenate([aC[c].transpose(0, 2, 1), an[c][:, :, None]], axis=2)
        in_maps.append({
            "xw": np.ascontiguousarray(xw), "xs": np.ascontiguousarray(xsm[c]),
            "w_in0": w_in0, "w_out0": w_out0, "w_in1": w_in1, "w_out1": w_out1,
            "cst": cst, "par": par,
            "bsbc": np.ascontiguousarray(np.broadcast_to(inp["odd_b_s"][0].reshape(1, 1024), (128, 1024)), f32),
            "vgbc": np.ascontiguousarray(np.broadcast_to(inp["odd_v_gain"][0][None, :], (128, 2048)), f32),
            "gfbc": np.ascontiguousarray(np.broadcast_to(np.asarray(inp["final_norm"])[None, :], (128, 2048)), f32),
            "wsT": np.ascontiguousarray(np.asarray(inp["odd_w_s"][0], f32).transpose(2, 0, 1)),
            "aCT": np.ascontiguousarray(aCT, f32),
            "ck": ck2, "cv": cv2,
            "ptb": np.ascontiguousarray(np.broadcast_to(pt[c][None, :], (128, 128)), np.int32),
        })
    res = run_bass_kernel_spmd(nc, in_maps, core_ids=list(range(NCR)))
    R = list(res.results)
    while len(R) < 8:
        R.append({k: np.zeros_like(v) for k, v in R[0].items()})
    y_prompt = np.stack([np.concatenate([R[2 * b]["y_p"], R[2 * b + 1]["y_p"]], 0) for b in range(4)])
    y_sample = np.stack([R[c]["y_s"] for c in range(8)])
    aCp = np.stack([R[2 * b + 1]["CTo_p"][:, :, :256].transpose(0, 2, 1) for b in range(4)])[None]
    anp_ = np.stack([R[2 * b + 1]["CTo_p"][:, :, 256] for b in range(4)])[None]
    amp = np.stack([R[2 * b + 1]["mo_p"][0] for b in range(4)])[None]
    aCs = np.stack([R[c]["CTo_s"][:, :, :256].transpose(0, 2, 1) for c in range(8)])[None]
    ans = np.stack([R[c]["CTo_s"][:, :, 256] for c in range(8)])[None]
    ams = np.stack([R[c]["mo_s"][0] for c in range(8)])[None]
    bkp = np.stack([np.concatenate([R[2 * b]["bk_p"], R[2 * b + 1]["bk_p"]], 0) for b in range(4)]).reshape(1, 4, 2048, 8, 128)
    bvp = np.stack([np.concatenate([R[2 * b]["bv_p"], R[2 * b + 1]["bv_p"]], 0) for b in range(4)]).reshape(1, 4, 2048, 8, 128)
    bks = np.stack([R[c]["bk_s"] for c in range(8)]).reshape(1, 8, 4, 8, 128)
    bvs = np.stack([R[c]["bv_s"] for c in range(8)]).reshape(1, 8, 4, 8, 128)
    cvs = np.stack([R[c]["cv_s"] for c in range(8)])[None]
    outs = (y_prompt, y_sample, aCp, anp_, amp, aCs, ans, ams, bkp, bvp, bks, bvs, cvs)
    return tuple(np.ascontiguousarray(o, dtype=np.float32) for o in outs)
```
